# Optimizing a Trainium2 kernel written in Bass

```python
import math
import jax, jax.numpy as jnp
from jax import lax
import numpy as np

D_MODEL = 2048
BATCH = 8
SEQ = 4096
DEPTH = 4

N_MIXERS = 2
Q_BLOCK = 128
D_FF = 4 * D_MODEL
RMS_EPS = 1e-6

A_HEADS = 32
A_QK_DIM = 128
A_V_DIM = 128
A_Q_RANK = 512
A_KV_RANK = 256
IDX_HEADS = 16
IDX_DIM = 128
IDX_TOPK = 256

B_HEADS = 16
B_HEAD_DIM = D_MODEL // B_HEADS

REL_BUCKETS = 32
REL_MAX_DIST = 128

N_A_LAYERS = (DEPTH + 1) // 2
N_B_LAYERS = DEPTH // 2
A_IN_COLS = A_Q_RANK + A_KV_RANK + IDX_DIM + IDX_HEADS
B_IN_COLS = 3 * B_HEADS * B_HEAD_DIM

kernel_name = "hybrid_dsa_stickbreaking_adaln_trunk"


def rmsnorm(x, g):
    xf = x.astype(jnp.float32)
    y = xf * lax.rsqrt(jnp.mean(xf * xf, axis=-1, keepdims=True) + RMS_EPS)
    return (y * g.astype(jnp.float32)).astype(x.dtype)


def layernorm(x, g):
    xf = x.astype(jnp.float32)
    mu = jnp.mean(xf, axis=-1, keepdims=True)
    var = jnp.mean(jnp.square(xf - mu), axis=-1, keepdims=True)
    return ((xf - mu) * lax.rsqrt(var + RMS_EPS) * g.astype(jnp.float32)).astype(x.dtype)


def modulate(h, shift, scale):
    return h * (1 + scale[:, None, :]) + shift[:, None, :]


def t5_bucket(dist):
    max_exact = REL_BUCKETS // 2
    d = jnp.maximum(dist, 1).astype(jnp.float32)
    large = max_exact + (jnp.log(d / max_exact) / math.log(REL_MAX_DIST / max_exact)
                         * (REL_BUCKETS - max_exact)).astype(jnp.int32)
    large = jnp.minimum(large, REL_BUCKETS - 1)
    return jnp.where(dist < max_exact, dist, large)


def dsa_attention(h, w_in, q_norm, kv_norm, idx_k_norm, w_qb, w_idx_qb, w_uk, w_uv, w_out, rel_bias):
    B, S, _ = h.shape
    k_top = min(IDX_TOPK, S // 4)
    proj = h @ w_in
    c1 = A_Q_RANK
    c2 = c1 + A_KV_RANK
    c3 = c2 + IDX_DIM
    q_lat, c_kv, k_idx, w_idx = jnp.split(proj, [c1, c2, c3], axis=-1)
    q_lat = rmsnorm(q_lat, q_norm)
    c_kv = rmsnorm(c_kv, kv_norm)
    k_idx = layernorm(k_idx, idx_k_norm)
    q = (q_lat @ w_qb).reshape(B, S, A_HEADS, A_QK_DIM)
    q_idx = (q_lat @ w_idx_qb).reshape(B, S, IDX_HEADS, IDX_DIM)
    w_idx = w_idx * (IDX_HEADS ** -0.5 * IDX_DIM ** -0.5)
    scale = A_QK_DIM ** -0.5
    key_pos = jnp.arange(S)

    def block(i):
        q0 = i * Q_BLOCK
        t = q0 + jnp.arange(Q_BLOCK)
        qb = lax.dynamic_slice_in_dim(q, q0, Q_BLOCK, axis=1)
        qib = lax.dynamic_slice_in_dim(q_idx, q0, Q_BLOCK, axis=1)
        wb = lax.dynamic_slice_in_dim(w_idx, q0, Q_BLOCK, axis=1)
        dots = jnp.einsum('bqhd,bsd->bqsh', qib, k_idx)
        score = jnp.einsum('bqsh,bqh->bqs', jax.nn.relu(dots), wb).astype(jnp.float32)
        score = jnp.where(key_pos[None, None, :] <= t[None, :, None], score, -jnp.inf)
        _, idx = lax.top_k(score, k_top)
        valid = idx <= t[None, :, None]
        kv_sel = jax.vmap(lambda kv, ix: kv[ix])(c_kv, idx)
        q_abs = jnp.einsum('bqhd,hcd->bqhc', qb, w_uk)
        logits = jnp.einsum('bqhc,bqkc->bqhk', q_abs, kv_sel).astype(jnp.float32) * scale
        bucket = t5_bucket(jnp.maximum(t[None, :, None] - idx, 0))
        logits = logits + jnp.moveaxis(rel_bias[bucket], -1, 2).astype(jnp.float32)
        logits = jnp.where(valid[:, :, None, :], logits, -jnp.inf)
        probs = jax.nn.softmax(logits, axis=-1).astype(h.dtype)
        o_lat = jnp.einsum('bqhk,bqkc->bqhc', probs, kv_sel)
        return jnp.einsum('bqhc,hcv->bqhv', o_lat, w_uv)

    out = lax.map(block, jnp.arange(S // Q_BLOCK))
    out = jnp.moveaxis(out, 0, 1).reshape(B, S, A_HEADS * A_V_DIM)
    return out @ w_out


def stick_breaking_attention(h, w_in, w_out):
    B, S, _ = h.shape
    q, k, v = jnp.split(h @ w_in, 3, axis=-1)
    q = q.reshape(B, S, B_HEADS, B_HEAD_DIM)
    k = k.reshape(B, S, B_HEADS, B_HEAD_DIM)
    v = v.reshape(B, S, B_HEADS, B_HEAD_DIM)
    scale = B_HEAD_DIM ** -0.5
    key_pos = jnp.arange(S)

    def block(i):
        q0 = i * Q_BLOCK
        t = q0 + jnp.arange(Q_BLOCK)
        qb = lax.dynamic_slice_in_dim(q, q0, Q_BLOCK, axis=1)
        z = jnp.einsum('bqhd,bshd->bhqs', qb, k).astype(jnp.float32) * scale
        strict = key_pos[None, :] < t[:, None]
        log_beta = jax.nn.log_sigmoid(z)
        log_rest = jnp.where(strict, log_beta - z, 0.0)
        later = lax.cumsum(log_rest, axis=3, reverse=True) - log_rest
        weights = jnp.where(strict, jnp.exp(log_beta + later), 0.0).astype(h.dtype)
        return jnp.einsum('bhqs,bshd->bqhd', weights, v)

    out = lax.map(block, jnp.arange(S // Q_BLOCK))
    out = jnp.moveaxis(out, 0, 1).reshape(B, S, B_HEADS * B_HEAD_DIM)
    return out @ w_out


def setup_inputs(seed: int = 0) -> dict:
    key = jax.random.key(seed)
    ks = jax.random.split(key, 20)

    def w(k, shape, fan_in):
        return jax.random.normal(k, shape, jnp.float32) * (fan_in ** -0.5)

    def gain(k, shape):
        return 1.0 + 0.05 * jax.random.normal(k, shape, jnp.float32)

    D = D_MODEL
    return {
        "x": jax.random.normal(ks[0], (BATCH, SEQ, D), jnp.float32),
        "c": jax.random.normal(ks[1], (BATCH, D), jnp.float32),
        "rel_bias": 0.5 * jax.random.normal(ks[2], (REL_BUCKETS, A_HEADS), jnp.float32),
        "ada_w": w(ks[3], (DEPTH, D, 6 * D), D),
        "ada_b": 0.01 * jax.random.normal(ks[4], (DEPTH, 6 * D), jnp.float32),
        "norm_g": gain(ks[5], (DEPTH, 4, D)),
        "a_w_in": w(ks[6], (N_A_LAYERS, D, A_IN_COLS), D),
        "a_q_norm": gain(ks[7], (N_A_LAYERS, A_Q_RANK)),
        "a_kv_norm": gain(ks[8], (N_A_LAYERS, A_KV_RANK)),
        "a_idx_k_norm": gain(ks[9], (N_A_LAYERS, IDX_DIM)),
        "a_w_qb": w(ks[10], (N_A_LAYERS, A_Q_RANK, A_HEADS * A_QK_DIM), A_Q_RANK),
        "a_w_idx_qb": w(ks[11], (N_A_LAYERS, A_Q_RANK, IDX_HEADS * IDX_DIM), A_Q_RANK),
        "a_w_uk": w(ks[12], (N_A_LAYERS, A_HEADS, A_KV_RANK, A_QK_DIM), A_KV_RANK),
        "a_w_uv": w(ks[13], (N_A_LAYERS, A_HEADS, A_KV_RANK, A_V_DIM), A_KV_RANK),
        "a_w_out": w(ks[14], (N_A_LAYERS, A_HEADS * A_V_DIM, D), A_HEADS * A_V_DIM),
        "b_w_in": w(ks[15], (N_B_LAYERS, D, B_IN_COLS), D),
        "b_w_out": w(ks[16], (N_B_LAYERS, B_HEADS * B_HEAD_DIM, D), B_HEADS * B_HEAD_DIM),
        "mlp_w1": w(ks[17], (DEPTH, D, D_FF), D),
        "mlp_w2": w(ks[18], (DEPTH, D_FF, D), D_FF),
    }


def reference(x, c, rel_bias, ada_w, ada_b, norm_g, a_w_in, a_q_norm, a_kv_norm, a_idx_k_norm,
              a_w_qb, a_w_idx_qb, a_w_uk, a_w_uv, a_w_out, b_w_in, b_w_out, mlp_w1, mlp_w2):
    mod = jnp.einsum('bd,lde->lbe', jax.nn.silu(c), ada_w) + ada_b[:, None, :]
    for layer in range(DEPTH):
        shift_m, scale_m, gate_m, shift_f, scale_f, gate_f = jnp.split(mod[layer], 6, axis=-1)
        j = layer // N_MIXERS
        h = modulate(rmsnorm(x, norm_g[layer, 0]), shift_m, scale_m)
        if layer % N_MIXERS == 0:
            y = dsa_attention(h, a_w_in[j], a_q_norm[j], a_kv_norm[j], a_idx_k_norm[j],
                              a_w_qb[j], a_w_idx_qb[j], a_w_uk[j], a_w_uv[j], a_w_out[j], rel_bias)
        else:
            y = stick_breaking_attention(h, b_w_in[j], b_w_out[j])
        x = x + gate_m[:, None, :] * rmsnorm(y, norm_g[layer, 1])
        h = modulate(rmsnorm(x, norm_g[layer, 2]), shift_f, scale_f)
        y = jnp.square(jax.nn.relu(h @ mlp_w1[layer])) @ mlp_w2[layer]
        x = x + gate_f[:, None, :] * rmsnorm(y, norm_g[layer, 3])
    return x
```

```python
import numpy as np
import ml_dtypes
from contextlib import ExitStack
import concourse.bass as bass
import concourse.mybir as mybir
from concourse.bass_utils import run_bass_kernel_spmd

F32 = mybir.dt.float32
BF16 = mybir.dt.bfloat16
ALU = mybir.AluOpType
AF = mybir.ActivationFunctionType
NPBF = ml_dtypes.bfloat16

D = 2048
DC = 16
DFF = 8192
T = 512
NEGFILL = -1.0e30
CAUSFILL = -2.0e30
EPS = 1e-6


class Buf:
    __slots__ = ("name", "w", "r", "psum")

    def __init__(self, name="", psum=False):
        self.name = name
        self.w = None
        self.r = {}
        self.psum = psum


class Sched:
    ENGS = ("pe", "act", "dve", "pool", "sp")

    def __init__(self, nc, stack, n_dma_sems=32, n_pool_sems=16):
        self.nc = nc
        self.dry = False
        self.prog = {e: [] for e in self.ENGS}
        self.sems = {}
        self.count = {}
        for e in ("pe", "act", "dve", "pool"):
            self.sems[e] = stack.enter_context(nc.semaphore("c_" + e))
            self.count[e] = 0
        self.ndma = {"sp": n_dma_sems, "pool": n_pool_sems}
        for q, n in self.ndma.items():
            for i in range(n):
                k = "%s%d" % (q, i)
                self.sems[k] = stack.enter_context(nc.semaphore(k))
                self.count[k] = 0
        self.dma_rr = {"sp": 0, "pool": 0}
        self.seen = {e: {} for e in self.ENGS}
        self.ninst = {e: 0 for e in self.ENGS}

    def _wait(self, eng, key, val):
        if val <= 0 or self.seen[eng].get(key, 0) >= val:
            return
        self.seen[eng][key] = val
        self.prog[eng].append(("wait", key, val))

    def _deps(self, eng, reads, writes, skip_self=False):
        deps = {}
        for b in reads:
            if b.w is not None:
                k, v = b.w
                if v > deps.get(k, 0):
                    deps[k] = v
            if b.psum:
                for k, v in b.r.items():
                    if k != eng and v > deps.get(k, 0):
                        deps[k] = v
        for b in writes:
            if b.w is not None:
                k, v = b.w
                if v > deps.get(k, 0):
                    deps[k] = v
            for k, v in b.r.items():
                if v > deps.get(k, 0):
                    deps[k] = v
        for k, v in deps.items():
            if skip_self and k == eng:
                continue
            self._wait(eng, k, v)

    def op(self, eng, fn, reads=(), writes=(), inc=True):
        if self.dry:
            return
        self._deps(eng, reads, writes, skip_self=(eng == "pe"))
        if inc:
            self.count[eng] += 1
            tok = (eng, self.count[eng])
            self.prog[eng].append(("inst", fn, eng, 1))
        else:
            tok = (eng, self.count[eng] + 1)
            self.prog[eng].append(("inst", fn, None, 0))
        self.ninst[eng] += 1
        for b in reads:
            if b.r.get(tok[0], 0) < tok[1]:
                b.r[tok[0]] = tok[1]
        for b in writes:
            b.w = tok
            b.r = {}

    def dma(self, out_ap, in_ap, reads=(), writes=(), queue="sp"):
        if self.dry:
            return
        i = self.dma_rr[queue]
        self.dma_rr[queue] = (i + 1) % self.ndma[queue]
        key = "%s%d" % (queue, i)
        self._wait(queue, key, self.count[key])
        self._deps(queue, reads, writes)
        self.count[key] += 16
        v = self.count[key]
        self.prog[queue].append(
            ("inst", (lambda e, o=out_ap, a=in_ap: e.dma_start(out=o, in_=a)), key, 16))
        self.ninst[queue] += 1
        for b in reads:
            b.r[key] = v
        for b in writes:
            b.w = (key, v)
            b.r = {}

    def barrier(self, engs=("pe", "act", "dve", "pool")):
        if self.dry:
            return
        for e in engs:
            for f in ("pe", "act", "dve", "pool"):
                if f != e:
                    self._wait(e, f, self.count[f])

    def wait_all(self, eng, bufs):
        if self.dry:
            return
        self._deps(eng, (), bufs)

    def replay(self):
        nc = self.nc
        engobj = {"pe": "tensor", "act": "scalar", "dve": "vector", "pool": "gpsimd", "sp": "sync"}
        with nc.Block() as block:
            for e in self.ENGS:
                prog = self.prog[e]
                if not prog:
                    continue

                def body(eng, prog=prog):
                    for item in prog:
                        if item[0] == "wait":
                            eng.wait_ge(self.sems[item[1]], item[2])
                        else:
                            ins = item[1](eng)
                            if item[2] is not None:
                                ins.then_inc(self.sems[item[2]], item[3])
                getattr(block, engobj[e])(body)


def blockify(W, ncols):
    K, N = W.shape
    return np.ascontiguousarray(
        W.reshape(K // 128, 128, N // ncols, ncols).transpose(2, 1, 0, 3))


def vec_pk(v):
    return np.ascontiguousarray(v.reshape(-1, 128).T)


def t5_bucket_np(dist):
    dist = np.asarray(dist)
    d = np.maximum(dist, 1).astype(np.float32)
    large = 16 + (np.log(d / 16) / np.float32(np.log(128 / 16)) * 16).astype(np.int32)
    large = np.minimum(large, 31)
    return np.where(dist < 16, dist, large)


class Cfg:
    def __init__(self, S_len=4096, layers=(0, 1, 2, 3), do_mixer=True, do_mlp=True):
        self.S = S_len
        self.NT = S_len // T
        self.NB = S_len // 128
        self.layers = tuple(layers)
        self.do_mixer = do_mixer
        self.do_mlp = do_mlp


def host_consts():
    c = {}
    c["ident"] = np.eye(128, dtype=np.float32).astype(NPBF)
    cm = np.zeros((128, 8, 128), np.float32)
    cm[:, 0, :] = 1.0
    cm[:, 1, :] = 1.0 / 2048
    cm[:, 2, :] = 1.0 / 512
    cm[:, 3, :] = 1.0 / 256
    cm[:, 4, :] = 1.0 / 128
    j = np.arange(128)[:, None]
    s = np.arange(128)[None, :]
    cm[:, 5, :] = -(j >= s).astype(np.float32)
    cm[:, 6, :] = -1.0
    c["cmats"] = cm.astype(NPBF)
    tt = np.arange(512)[None, None, :]
    mm = np.arange(4)[None, :, None]
    ss = np.arange(128)[:, None, None]
    c["sbmask"] = ((tt - 128 * mm) > ss).astype(np.float32).astype(NPBF)
    c["causneg"] = np.where(s > j, CAUSFILL, 0.0).astype(np.float32)
    return c


def host_prepare(inputs, cfg):
    f32 = np.float32
    sh = {}
    sh.update(host_consts())
    L = 4
    ada_w = inputs["ada_w"]
    sh["adaw"] = np.stack([blockify(ada_w[l], 512) for l in range(L)])
    sh["adab"] = np.ascontiguousarray(
        inputs["ada_b"].reshape(L, 96, 128).transpose(2, 0, 1).reshape(128, L * 96))
    sh["normg"] = np.ascontiguousarray(
        inputs["norm_g"].reshape(L, 4, 16, 128).transpose(3, 0, 1, 2).reshape(128, L * 64))
    sh["w1"] = np.stack([blockify(inputs["mlp_w1"][l], 512) for l in range(L)])
    sh["w2"] = np.stack([blockify(inputs["mlp_w2"][l], 128) for l in range(L)])
    sh["bwin"] = np.stack([blockify(inputs["b_w_in"][i], 512) for i in range(2)])
    sh["bwout"] = np.stack([blockify(inputs["b_w_out"][i], 512) for i in range(2)])
    awin = np.zeros((2, 2048, 1024), f32)
    awin[:, :, :912] = inputs["a_w_in"]
    sh["awin"] = np.stack([blockify(awin[i], 512) for i in range(2)])
    sh["awqb"] = np.stack([blockify(inputs["a_w_qb"][i], 1024) for i in range(2)])
    sh["awiqb"] = np.stack([blockify(inputs["a_w_idx_qb"][i], 2048) for i in range(2)])
    wuk = inputs["a_w_uk"]
    sh["awuk"] = np.ascontiguousarray(
        wuk.reshape(2, 4, 8, 256, 128).transpose(0, 1, 4, 2, 3))
    wuv = inputs["a_w_uv"]
    sh["awuv"] = np.ascontiguousarray(
        wuv.reshape(2, 4, 8, 2, 128, 128).transpose(0, 1, 4, 2, 3, 5))
    sh["awg"] = np.ascontiguousarray(np.concatenate(
        [sh.pop("awqb").reshape(2, 4, 128, 4096), sh.pop("awuk").reshape(2, 4, 128, 2048),
         sh.pop("awuv").reshape(2, 4, 128, 2048)], axis=3))
    sh["awout"] = np.stack([blockify(inputs["a_w_out"][i], 256) for i in range(2)])
    sm = np.zeros((128, 2, 8), f32)
    for i in range(2):
        sm[:, i, 0:4] = vec_pk(inputs["a_q_norm"][i])
        sm[:, i, 4:6] = vec_pk(inputs["a_kv_norm"][i])
        sm[:, i, 6:7] = vec_pk(inputs["a_idx_k_norm"][i])
    sh["asmall"] = sm.reshape(128, 16)
    rb = inputs["rel_bias"]
    s_ = np.arange(128)[:, None]
    t_ = np.arange(128)[None, :]
    b0 = t5_bucket_np(np.maximum(t_ - s_, 0))
    b1 = t5_bucket_np(t_ - s_ + 128)
    rbt = np.zeros((128, 32, 3, 128), f32)
    rbt[:, :, 0, :] = rb[b0].transpose(0, 2, 1)
    rbt[:, :, 1, :] = rb[b1].transpose(0, 2, 1)
    rbt[:, :, 2, :] = rb[31][None, :, None]
    sh["rbt"] = np.ascontiguousarray(rbt.reshape(128, 4, 8 * 3 * 128))
    sh["cbias"] = np.ascontiguousarray(np.broadcast_to(rb[31][None, :], (128, 32))).astype(f32)
    percore = []
    for b in range(inputs["x"].shape[0]):
        pc = {}
        pc["xT"] = np.ascontiguousarray(inputs["x"][b, :cfg.S].T)
        pc["cT"] = vec_pk(inputs["c"][b])
        percore.append(pc)
    return sh, percore


class Builder:
    def __init__(self, cfg):
        self.cfg = cfg
        self.nc = bass.Bass("TRN2", target_bir_lowering=False)
        self.stack = ExitStack()
        self.S = Sched(self.nc, self.stack)
        self.wplan = []
        self.wpos = 0
        self.wissued = 0

    def din(self, name, shape, dt=F32):
        return self.nc.dram_tensor(name, list(shape), dt, kind="ExternalInput").ap()

    def dscr(self, name, shape, dt):
        return self.nc.dram_tensor(name, list(shape), dt, kind="Internal").ap()

    def declare(self):
        cfg = self.cfg
        S_ = cfg.S
        d = {}
        d["xT"] = self.din("xT", [D, S_])
        d["cT"] = self.din("cT", [128, 16])
        d["ident"] = self.din("ident", [128, 128], BF16)
        d["cmats"] = self.din("cmats", [128, 8, 128], BF16)
        d["sbmask"] = self.din("sbmask", [128, 4, 512], BF16)
        d["causneg"] = self.din("causneg", [128, 128])
        d["adaw"] = self.din("adaw", [4, 24, 128, 16, 512])
        d["adab"] = self.din("adab", [128, 384])
        d["normg"] = self.din("normg", [128, 256])
        d["w1"] = self.din("w1", [4, 16, 128, 16, 512])
        d["w2"] = self.din("w2", [4, 16, 128, 64, 128])
        d["bwin"] = self.din("bwin", [2, 12, 128, 16, 512])
        d["bwout"] = self.din("bwout", [2, 4, 128, 16, 512])
        d["awin"] = self.din("awin", [2, 2, 128, 16, 512])
        d["awg"] = self.din("awg", [2, 4, 128, 8192])
        d["awiqb"] = self.din("awiqb", [2, 1, 128, 4, 2048])
        d["awout"] = self.din("awout", [2, 8, 128, 32, 256])
        d["asmall"] = self.din("asmall", [128, 16])
        d["rbt"] = self.din("rbt", [128, 4, 8 * 3 * 128])
        d["cbias"] = self.din("cbias", [128, 32])
        self.d = d
        self.yT = self.nc.dram_tensor("yT", [D, S_], F32, kind="ExternalOutput").ap()
        self.xs = self.dscr("xs", [D, S_], F32)
        mk = lambda nm: [[Buf("%s_%d_%d" % (nm, j, ch)) for ch in range(DC)] for j in range(cfg.NT)]
        self.xs_buf = mk("xs")
        self.xin_buf = mk("xin")
        self.yT_buf = mk("yT")
        self.wb = {}
        self.wb_buf = {}
        for name in ("w1", "w2", "bwin", "bwout", "awin", "awg", "awiqb", "awout"):
            shp = list(d[name].shape)
            self.wb[name] = self.dscr(name + "_bf", shp, BF16)
            self.wb_buf[name] = [[] for i in range(shp[0])]
        self.qc = self.dscr("qcache", [16, 128, S_], BF16)
        self.kc = self.dscr("kcache", [16, 128, S_], BF16)
        self.vc = self.dscr("vcache", [16, 128, cfg.NB, 128], BF16)
        self.ac = self.dscr("acache", [16, 128, S_], BF16)
        self.qc_buf, self.kc_buf, self.vc_buf, self.ac_buf = Buf("qc"), Buf("kc"), Buf("vc"), Buf("ac")

    def alloc(self):
        nc = self.nc
        self.ARENA_BYTES = 207 * 1024
        self.arena = self.stack.enter_context(nc.sbuf_tensor("arena", [128, self.ARENA_BYTES // 2], BF16))
        self.ps = []
        self.psb = []
        for i in range(8):
            self.ps.append(self.stack.enter_context(nc.psum_tensor("ps%d" % i, [128, 512], F32)))
            self.psb.append(Buf("ps%d" % i, psum=True))

    def carve(self, off_bytes, shape, dt):
        esz = 4 if dt == F32 else 2
        n = int(np.prod(shape[1:])) * esz
        assert off_bytes % 4 == 0 and off_bytes + n <= self.ARENA_BYTES, (off_bytes, n)
        a = self.arena[:, off_bytes // 2:(off_bytes + n) // 2]
        if dt == F32:
            a = a.bitcast(F32)
        if len(shape) == 3:
            a = a.rearrange("p (a b) -> p a b", a=shape[1])
        elif len(shape) == 4:
            a = a.rearrange("p (a b c) -> p a b c", a=shape[1], b=shape[2])
        return a

    def wnext(self, dram_ap, buf, shape, queue="sp"):
        if self.S.dry:
            self.wplan.append((dram_ap, buf, shape, queue))
            return None, None
        n = self.wpos
        self.wpos += 1
        while self.wissued < min(len(self.wplan), n + self.WDEPTH):
            i = self.wissued
            ap_i, buf_i, shp_i, q_i = self.wplan[i]
            slot = i % self.NSLOT
            view = self.carve(self.RING_OFF + slot * 16384, [128] + list(shp_i), BF16)
            self.S.dma(view, ap_i, reads=list(buf_i), writes=[self.ring_buf[slot]], queue=q_i)
            self.wissued += 1
        slot = n % self.NSLOT
        shp = self.wplan[n][2]
        return self.carve(self.RING_OFF + slot * 16384, [128] + list(shp), BF16), self.ring_buf[slot]

    def mm(self, ps_i, lhsT, rhs, rd, start, stop, inc=None, out=None, sgc=False):
        o = self.ps[ps_i][:] if out is None else out
        if inc is None:
            inc = True
        if sgc:
            self.S.op("pe", lambda e: e.matmul(o, lhsT, rhs, start=start, stop=stop, skip_group_check=True),
                      reads=rd, writes=[self.psb[ps_i]], inc=inc)
        else:
            self.S.op("pe", lambda e: e.matmul(o, lhsT, rhs, start=start, stop=stop),
                      reads=rd, writes=[self.psb[ps_i]], inc=inc)

    def cast_weights(self, name, idx):
        src = self.d[name][idx]
        dst = self.wb[name][idx]
        nd = len(src.shape)
        if nd == 4:
            a, b, c, e = src.shape
            src2 = src.rearrange("a b c e -> (a b) (c e)")
            dst2 = dst.rearrange("a b c e -> (a b) (c e)")
        elif nd == 5:
            src2 = src.rearrange("a b c e f -> (a b) (c e f)")
            dst2 = dst.rearrange("a b c e f -> (a b) (c e f)")
        else:
            src2 = src.rearrange("a b c -> (a b) c")
            dst2 = dst.rearrange("a b c -> (a b) c")
        rows = src2.shape[0]
        step = 128 * max(1, (8 << 20) // (128 * src2.shape[1] * 4))
        for r0 in range(0, rows, step):
            r1 = min(rows, r0 + step)
            bf = Buf("%s_%d_%d" % (name, idx, r0))
            self.wb_buf[name][idx].append(bf)
            self.S.dma(dst2[r0:r1], src2[r0:r1], writes=[bf], queue="pool")

    def setup_layout(self):
        c = self.carve
        self.ident = c(0, [128, 128], BF16)
        self.cmats = c(256, [128, 8, 128], BF16)
        self.ones1 = self.cmats[:, 0, :]
        self.onesD = self.cmats[:, 1, :]
        self.ones512 = self.cmats[:, 2, :]
        self.ones256 = self.cmats[:, 3, :]
        self.ones128 = self.cmats[:, 4, :]
        self.negU = self.cmats[:, 5, :]
        self.negOnes = self.cmats[:, 6, :]
        self.sbmask = c(2304, [128, 4, 512], BF16)
        self.causneg = c(6400, [128, 128], F32)
        self.cT = c(6912, [128, 16], F32)
        self.csil = c(6976, [128, 16], BF16)
        self.adab = c(7008, [128, 384], F32)
        self.normg = c(8544, [128, 256], F32)
        self.modv = c(9568, [128, 384], F32)
        self.lvec = c(11104, [128, 6, 16], F32)
        self.asmall = c(11488, [128, 16], F32)
        self.cbias = c(11552, [128, 32], F32)
        self.ltmp = c(11680, [128, 16], F32)
        self.cbuf = Buf("consts")
        self.modv_buf = Buf("modv")
        self.lvec_buf = Buf("lvec")
        self.RING_OFF = 12288
        self.NSLOT = 3
        self.WDEPTH = 3
        self.ring_buf = [Buf("ring%d" % i) for i in range(self.NSLOT)]
        self.WORK = self.RING_OFF + self.NSLOT * 16384

    def emit_init(self):
        S = self.S
        d = self.d
        for dst, src in ((self.ident, d["ident"]), (self.cmats, d["cmats"]), (self.sbmask, d["sbmask"]),
                         (self.causneg, d["causneg"]), (self.cT, d["cT"]), (self.adab, d["adab"]),
                         (self.normg, d["normg"]), (self.asmall, d["asmall"]), (self.cbias, d["cbias"])):
            S.dma(dst, src, writes=[self.cbuf])
        S.op("act", lambda e: e.activation(self.csil, self.cT, AF.Silu), reads=[self.cbuf], writes=[self.cbuf])
        S.barrier()

    def emit_ada(self, l):
        S = self.S
        bank = 6
        for blk in range(24):
            Wv, Wb = self.wnext(self.d["adaw"][l, blk], [], [16, 512], queue="pool")
            if S.dry:
                continue
            for oc in range(4):
                col = blk * 4 + oc
                for kc in range(16):
                    self.mm(bank, Wv[:, kc, oc * 128:(oc + 1) * 128], self.csil[:, kc:kc + 1],
                            [Wb, self.cbuf], start=(kc == 0), stop=(kc == 15), inc=(kc == 15),
                            out=self.ps[bank][:, col:col + 1])
        S.op("dve", lambda e: e.tensor_tensor(self.modv[:, l * 96:(l + 1) * 96], self.ps[bank][:, 0:96],
                                              self.adab[:, l * 96:(l + 1) * 96], ALU.add),
             reads=[self.psb[bank], self.cbuf], writes=[self.modv_buf])

    def emit_layer_vecs(self, l):
        S = self.S
        mv = lambda m: self.modv[:, l * 96 + m * 16: l * 96 + (m + 1) * 16]
        g = lambda i: self.normg[:, (l * 4 + i) * 16:(l * 4 + i + 1) * 16]
        lv = self.lvec
        rd = [self.modv_buf, self.cbuf]
        wr = [self.lvec_buf]
        for (dst, sc, gi) in ((0, 1, 0), (3, 4, 2)):
            S.op("dve", lambda e, sc=sc: e.tensor_scalar(self.ltmp, mv(sc), 1.0, None, ALU.add), reads=rd, writes=wr)
            S.op("dve", lambda e, dst=dst, gi=gi: e.tensor_tensor(lv[:, dst, :], self.ltmp, g(gi), ALU.mult), reads=rd + wr, writes=wr)
        for (dst, sh) in ((1, 0), (4, 3)):
            S.op("dve", lambda e, dst=dst, sh=sh: e.tensor_copy(lv[:, dst, :], mv(sh)), reads=rd, writes=wr)
        for (dst, gt, gi) in ((2, 2, 1), (5, 5, 3)):
            S.op("dve", lambda e, dst=dst, gt=gt, gi=gi: e.tensor_tensor(lv[:, dst, :], mv(gt), g(gi), ALU.mult), reads=rd, writes=wr)

    def xview(self, ap, t0, n=T):
        return ap.rearrange("(c p) t -> p c t", p=128)[:, :, t0:t0 + n]

    def small_layout(self, base):
        c = self.carve
        o = self.WORK + base
        self.SQ = [c(o + i * 1024, [128, 512], BF16) for i in range(3)]
        self.SQb = [Buf("sq%d" % i) for i in range(3)]
        o += 3072
        self.RS = c(o, [128, 512], F32)
        self.RSb = Buf("rs")
        o += 2048
        self.T1 = [c(o + i * 2048, [128, 512], F32) for i in range(2)]
        self.T1b = [Buf("t1%d" % i) for i in range(2)]
        o += 4096
        self.XC = [c(o + i * 2048, [128, 512], F32) for i in range(4)]
        self.XCb = [Buf("xc%d" % i) for i in range(4)]
        o += 8192
        self.RL = [c(o + i * 1024, [128, 512], BF16) for i in range(2)]
        self.RLb = [Buf("rl%d" % i) for i in range(2)]
        o += 2048
        self.rr = {"sq": 0, "t1": 0, "xc": 0, "rl": 0, "ps": 0}
        return o - self.WORK

    def rot(self, key, n):
        i = self.rr[key]
        self.rr[key] = (i + 1) % n
        return i

    def rstd_from_psum(self, bank):
        S = self.S
        XC, SQ, T1, RL, RS = self.XC, self.SQ, self.T1, self.RL, self.RS
        S.op("act", lambda e: e.activation(RS, self.ps[bank][:], AF.Ln, bias=EPS),
             reads=[self.psb[bank]], writes=[self.RSb])
        S.op("act", lambda e: e.activation(RS, RS, AF.Exp, scale=-0.5), reads=[self.RSb], writes=[self.RSb])

    def norm_front(self, xsrc, xsrc_buf, t0, avec, bvec, H, Hb, bank=7):
        S = self.S
        XC, SQ, T1, RL, RS = self.XC, self.SQ, self.T1, self.RL, self.RS
        xv = self.xview(xsrc, t0)
        for ch in range(DC):
            k = self.rot("xc", 4)
            S.dma(XC[k], xv[:, ch, :], reads=[xsrc_buf[t0 // T][ch]], writes=[self.XCb[k]])
            q = self.rot("sq", 3)
            S.op("act", lambda e, k=k, q=q: e.activation(SQ[q], XC[k], AF.Square),
                 reads=[self.XCb[k]], writes=[self.SQb[q]])
            self.mm(bank, self.onesD, SQ[q], [self.SQb[q], self.cbuf], start=(ch == 0), stop=(ch == DC - 1))
        self.rstd_from_psum(bank)
        for ch in range(DC):
            k = self.rot("xc", 4)
            S.dma(XC[k], xv[:, ch, :], reads=[xsrc_buf[t0 // T][ch]], writes=[self.XCb[k]])
            i = self.rot("t1", 2)
            S.op("dve", lambda e, k=k, i=i, ch=ch: e.scalar_tensor_tensor(
                T1[i], XC[k], avec[:, ch:ch + 1], RS, ALU.mult, ALU.mult),
                reads=[self.XCb[k], self.RSb, self.lvec_buf], writes=[self.T1b[i]])
            S.op("act", lambda e, i=i, ch=ch: e.activation(H[:, ch, :], T1[i], AF.Identity, bias=bvec[:, ch:ch + 1]),
                 reads=[self.T1b[i], self.lvec_buf], writes=[Hb])

    def resid_back(self, Y, Yb, bank, gvec, xsrc, xsrc_buf, xdst, xdst_buf, t0):
        S = self.S
        XC, SQ, T1, RL, RS = self.XC, self.SQ, self.T1, self.RL, self.RS
        self.rstd_from_psum(bank)
        xv = self.xview(xsrc, t0)
        ov = self.xview(xdst, t0)
        for ch in range(DC):
            k = self.rot("xc", 4)
            S.dma(XC[k], xv[:, ch, :], reads=[xsrc_buf[t0 // T][ch]], writes=[self.XCb[k]])
            i = self.rot("t1", 2)
            S.op("dve", lambda e, i=i, ch=ch: e.scalar_tensor_tensor(
                T1[i], Y[:, ch, :], gvec[:, ch:ch + 1], RS, ALU.mult, ALU.mult),
                reads=[Yb, self.RSb, self.lvec_buf], writes=[self.T1b[i]])
            S.op("dve", lambda e, i=i, k=k: e.tensor_tensor(XC[k], XC[k], T1[i], ALU.add),
                 reads=[self.T1b[i], self.XCb[k]], writes=[self.XCb[k]])
            S.dma(ov[:, ch, :], XC[k], reads=[self.XCb[k]], writes=[xdst_buf[t0 // T][ch]])

    def out_proj(self, wname, widx, nblk, ncol, KC, A, Ab, Y, Yb, nbank=7):
        S = self.S
        XC, SQ, T1, RL, RS = self.XC, self.SQ, self.T1, self.RL, self.RS
        noc = ncol // 128
        pend = None
        oc_g = 0
        for blk in range(nblk):
            Wv, Wb = self.wnext(self.wb[wname][widx, blk], self.wb_buf[wname][widx], [KC, ncol])
            if S.dry:
                continue
            for oc in range(noc):
                bank = self.rot("ps", 4)
                for kc in range(KC):
                    self.mm(bank, Wv[:, kc, oc * 128:(oc + 1) * 128], A[:, kc, :], [Wb, Ab],
                            start=(kc == 0), stop=(kc == KC - 1), inc=(kc == KC - 1))
                dbg = getattr(self.cfg, "dbg", "")
                if pend is not None and "noones" not in dbg:
                    self.mm(nbank, self.onesD, SQ[pend[0]], [self.SQb[pend[0]], self.cbuf],
                            start=(pend[1] == 0), stop=False)
                q = self.rot("sq", 3)
                if "nocopy" not in dbg:
                    S.op("dve", lambda e, bank=bank, o=oc_g: e.tensor_copy(Y[:, o, :], self.ps[bank][:]),
                         reads=[self.psb[bank]], writes=[Yb])
                if "nosq" not in dbg:
                    S.op("act", lambda e, o=oc_g, q=q: e.activation(SQ[q], Y[:, o, :], AF.Square),
                         reads=[Yb], writes=[self.SQb[q]])
                pend = (q, oc_g)
                oc_g += 1
        if pend is not None and "noones" not in getattr(self.cfg, "dbg", ""):
            self.mm(nbank, self.onesD, SQ[pend[0]], [self.SQb[pend[0]], self.cbuf],
                    start=(pend[1] == 0), stop=True)

    def emit_mlp_pass(self, l, xsrc, xsrc_buf, xdst, xdst_buf):
        S = self.S
        cfg = self.cfg
        c = self.carve
        W0 = self.WORK
        Y = c(W0, [128, 16, 512], F32)
        H = c(W0 + 32768, [128, 16, 512], BF16)
        G = c(W0 + 49152, [128, 64, 512], BF16)
        self.small_layout(114688)
        XC, SQ, T1, RL, RS = self.XC, self.SQ, self.T1, self.RL, self.RS
        Yb, Hb, Gb = Buf("Y"), Buf("H"), Buf("G")
        lv = self.lvec
        for j in range(cfg.NT):
            t0 = j * T
            dbg = getattr(cfg, "dbg", "")
            self.norm_front(xsrc, xsrc_buf, t0, lv[:, 3, :], lv[:, 4, :], H, Hb)
            if "mlp_front" in dbg:
                continue
            for blk in range(16):
                Wv, Wb = self.wnext(self.wb["w1"][l, blk], self.wb_buf["w1"][l], [16, 512])
                if S.dry:
                    continue
                for oc in range(4):
                    bank = self.rot("ps", 4)
                    for kc in range(16):
                        self.mm(bank, Wv[:, kc, oc * 128:(oc + 1) * 128], H[:, kc, :], [Wb, Hb],
                                start=(kc == 0), stop=(kc == 15), inc=(kc == 15))
                    r = self.rot("rl", 2)
                    S.op("act", lambda e, bank=bank, r=r: e.activation(RL[r], self.ps[bank][:], AF.Relu),
                         reads=[self.psb[bank]], writes=[self.RLb[r]])
                    S.op("dve", lambda e, r=r, hc=blk * 4 + oc: e.tensor_tensor(G[:, hc, :], RL[r], RL[r], ALU.mult),
                         reads=[self.RLb[r]], writes=[Gb])
            if "mlp_up" in dbg:
                continue
            self.out_proj("w2", l, 16, 128, 64, G, Gb, Y, Yb)
            if "mlp_down" in dbg:
                continue
            self.resid_back(Y, Yb, 7, lv[:, 5, :], xsrc, xsrc_buf, xdst, xdst_buf, t0)

    def full_barrier(self):
        S = self.S
        if S.dry:
            return
        for e in S.ENGS:
            for f in ("pe", "act", "dve", "pool"):
                if f != e:
                    S._wait(e, f, S.count[f])
            for i in range(S.ndma["sp"]):
                k = "sp%d" % i
                S._wait(e, k, S.count[k])

    def layer_weight_names(self, l):
        if l % 2 == 0:
            return [("awin", l // 2), ("awiqb", l // 2), ("awg", l // 2), ("awout", l // 2), ("w1", l), ("w2", l)]
        return [("bwin", l // 2), ("bwout", l // 2), ("w1", l), ("w2", l)]

    def emit_casts(self, l):
        if self.S.dry:
            return
        cfg = self.cfg
        for name, idx in self.layer_weight_names(l):
            if name in ("w1", "w2") and not cfg.do_mlp:
                continue
            if name not in ("w1", "w2") and not cfg.do_mixer:
                continue
            self.cast_weights(name, idx)

    def emit_all(self):
        cfg = self.cfg
        S = self.S
        self.emit_init()
        dbg = getattr(cfg, "dbg", "")
        if "nocast" not in dbg:
            self.emit_casts(cfg.layers[0])
        if "noada" not in dbg:
            for l in cfg.layers:
                self.emit_ada(l)
        if "stop" in dbg:
            if not S.dry:
                for lst in self.wb_buf.values():
                    for l2 in lst:
                        S.wait_all("pool", l2)
                S.wait_all("sp", [self.modv_buf])
            return
        passes = []
        for l in cfg.layers:
            if cfg.do_mixer:
                passes.append(("mix", l))
            if cfg.do_mlp:
                passes.append(("mlp", l))
        src, srcb = self.d["xT"], self.xin_buf
        cur_layer = None
        for pi, (kind, l) in enumerate(passes):
            last = pi == len(passes) - 1
            dst, dstb = (self.yT, self.yT_buf) if last else (self.xs, self.xs_buf)
            self.full_barrier()
            if l != cur_layer:
                cur_layer = l
                self.emit_layer_vecs(l)
                li = cfg.layers.index(l)
                if li + 1 < len(cfg.layers):
                    self.emit_casts(cfg.layers[li + 1])
            if kind == "mlp":
                self.emit_mlp_pass(l, src, srcb, dst, dstb)
            elif l % 2 == 0:
                self.emit_dsa_pass(l, src, srcb, dst, dstb)
            else:
                self.emit_sb_pass(l, src, srcb, dst, dstb)
            src, srcb = dst, dstb
        if not S.dry:
            S.wait_all("sp", [b for row in self.yT_buf for b in row])
            self.full_barrier()

    def build(self):
        self.declare()
        self.alloc()
        self.setup_layout()
        self.S.dry = True
        self.emit_all()
        self.S.dry = False
        self.rr = None
        self.emit_all()
        self.S.replay()
        import os
        if os.environ.get("DUMP"):
            open(os.environ["DUMP"], "w").write(self.nc.concise_text())
        self.stack.close()
        return self.nc


def run_cfg(inputs, cfg, core_ids):
    sh, percore = host_prepare(inputs, cfg)
    b = Builder(cfg)
    nc = b.build()
    in_maps = []
    for c in core_ids:
        m = dict(sh)
        m.update(percore[c])
        in_maps.append(m)
    res = run_bass_kernel_spmd(nc, in_maps, core_ids=list(range(len(core_ids))))
    outs = [np.ascontiguousarray(r["yT"].T) for r in res.results]
    return np.stack(outs), b


def kernel(**inputs):
    inputs = {k: np.asarray(v) for k, v in inputs.items()}
    cfg = Cfg()
    out, _ = run_cfg(inputs, cfg, list(range(8)))
    return out.astype(np.float32)


def _emit_sb_pass(self, l, xsrc, xsrc_buf, xdst, xdst_buf):
    S = self.S
    cfg = self.cfg
    c = self.carve
    W0 = self.WORK
    i_sb = l // 2
    lv = self.lvec
    NT, NB, SL = cfg.NT, cfg.NB, cfg.S
    scale = float(128 ** -0.5)
    if not hasattr(self, "sbc_buf"):
        self.sbc_buf = {nm: [Buf("%s%d" % (nm, j)) for j in range(NT)] for nm in ("q", "k", "v", "a")}
    cb = self.sbc_buf
    H = c(W0, [128, 16, 512], BF16)
    QT = c(W0 + 16384, [128, 16, 512], BF16)
    KT = c(W0 + 32768, [128, 16, 512], BF16)
    VT = c(W0 + 49152, [128, 16, 4, 128], BF16)
    self.small_layout(65536)
    Hb, QTb, KTb, VTb = Buf("H"), Buf("QT"), Buf("KT"), Buf("VT")
    for j in range(NT):
        t0 = j * T
        self.norm_front(xsrc, xsrc_buf, t0, lv[:, 0, :], lv[:, 1, :], H, Hb)
        for blk in range(12):
            Wv, Wb = self.wnext(self.wb["bwin"][i_sb, blk], self.wb_buf["bwin"][i_sb], [16, 512])
            if S.dry:
                continue
            if blk < 8:
                for oc in range(4):
                    head = (blk % 4) * 4 + oc
                    bank = self.rot("ps", 4)
                    for kc in range(16):
                        self.mm(bank, Wv[:, kc, oc * 128:(oc + 1) * 128], H[:, kc, :], [Wb, Hb],
                                start=(kc == 0), stop=(kc == 15), inc=(kc == 15))
                    if blk < 4:
                        S.op("act", lambda e, bank=bank, head=head: e.activation(QT[:, head, :], self.ps[bank][:], AF.Identity, scale=scale),
                             reads=[self.psb[bank]], writes=[QTb])
                    else:
                        S.op("dve", lambda e, bank=bank, head=head: e.tensor_copy(KT[:, head, :], self.ps[bank][:]),
                             reads=[self.psb[bank]], writes=[KTb])
            else:
                g = blk - 8
                for tb in range(4):
                    bank = self.rot("ps", 4)
                    for kc in range(16):
                        self.mm(bank, H[:, kc, tb * 128:(tb + 1) * 128], Wv[:, kc, :], [Wb, Hb],
                                start=(kc == 0), stop=(kc == 15), inc=(kc == 15))
                    pv = self.ps[bank][:].rearrange("p (h d) -> p h d", h=4)
                    if tb % 2 == 0:
                        S.op("act", lambda e, pv=pv, g=g, tb=tb: e.copy(VT[:, g * 4:(g + 1) * 4, tb, :], pv),
                             reads=[self.psb[bank]], writes=[VTb])
                    else:
                        S.op("dve", lambda e, pv=pv, g=g, tb=tb: e.tensor_copy(VT[:, g * 4:(g + 1) * 4, tb, :], pv),
                             reads=[self.psb[bank]], writes=[VTb])
        S.dma(self.qc[:, :, t0:t0 + T].rearrange("h d t -> d h t"), QT, reads=[QTb], writes=[cb["q"][j]])
        S.dma(self.kc[:, :, t0:t0 + T].rearrange("h d t -> d h t"), KT, reads=[KTb], writes=[cb["k"][j]])
        S.dma(self.vc[:, :, 4 * j:4 * j + 4, :].rearrange("h p b d -> p h b d"), VT, reads=[VTb], writes=[cb["v"][j]])
    self.full_barrier()
    dbg = getattr(cfg, "dbg", "")
    if "sb1" in dbg:
        return
    G_ = 2
    slots = []
    for i in range(4):
        o = W0 + i * 32768
        slots.append(dict(K=c(o, [128, SL], BF16) if SL * 2 <= 8192 else None,
                          V=c(o + 8192, [128, NB, 128], BF16), Q=c(o + 16384, [128, SL], BF16),
                          A=c(o + 24576, [128, SL], BF16),
                          Kb=Buf("K%d" % i), Vb=Buf("V%d" % i), Qb=Buf("Q%d" % i), Ab=Buf("A%d" % i)))
    o = W0 + 131072
    Et = [c(o + i * 2048, [128, 512], F32) for i in range(2)]
    Etb = [Buf("E%d" % i) for i in range(2)]
    o += 4096
    SPt = [c(o + i * 1024, [128, 512], BF16) for i in range(4)]
    SPb = [Buf("SP%d" % i) for i in range(4)]
    o += 4096
    Wt = [c(o + i * 1024, [128, 512], BF16) for i in range(4)]
    Wtb = [Buf("W%d" % i) for i in range(4)]
    o += 4096
    Rt = [c(o + i * 1024, [128, 512], BF16) for i in range(2)]
    Rtb = [Buf("R%d" % i) for i in range(2)]
    ngroups = 16 // G_

    def load_group(g):
        for hh in range(G_):
            h = g * G_ + hh
            sl = slots[(g % 2) * G_ + hh]
            S.dma(sl["K"], self.kc[h], reads=cb["k"], writes=[sl["Kb"]])
            S.dma(sl["V"], self.vc[h], reads=cb["v"], writes=[sl["Vb"]])
            S.dma(sl["Q"], self.qc[h], reads=cb["q"], writes=[sl["Qb"]])

    load_group(0)
    for g in range(ngroups):
        if g + 1 < ngroups:
            load_group(g + 1)
        sls = [slots[(g % 2) * G_ + hh] for hh in range(G_)]
        for j in range(NT):
            t0 = j * T
            steps = list(range(4 * j + 3, -1, -1))

            def mm1(k, hh):
                sb = steps[k]
                sl = sls[hh]
                bank = hh * 2 + (k % 2)
                self.mm(bank, sl["K"][:, sb * 128:(sb + 1) * 128], sl["Q"][:, t0:t0 + T], [sl["Kb"], sl["Qb"]],
                        start=True, stop=True)

            for hh in range(G_):
                mm1(0, hh)
            for k, sb in enumerate(steps):
                m = sb - 4 * j
                for hh in range(G_):
                    bank = hh * 2 + (k % 2)
                    si = hh * 2 + (k % 2)
                    S.op("act", lambda e, hh=hh, bank=bank: e.activation(Et[hh], self.ps[bank][:], AF.Exp),
                         reads=[self.psb[bank]], writes=[Etb[hh]])
                    S.op("act", lambda e, hh=hh, si=si: e.activation(SPt[si], Et[hh], AF.Ln, bias=1.0),
                         reads=[Etb[hh]], writes=[SPb[si]])
                    if m >= 0:
                        S.op("dve", lambda e, si=si, m=m: e.tensor_tensor(SPt[si], SPt[si], self.sbmask[:, m, :], ALU.mult),
                             reads=[SPb[si], self.cbuf], writes=[SPb[si]])
                for hh in range(G_):
                    bank = hh * 2 + (k % 2)
                    si = hh * 2 + (k % 2)
                    self.mm(bank, self.negU, SPt[si], [SPb[si], self.cbuf], start=False, stop=(k == 0), sgc=True)
                    if k > 0:
                        self.mm(bank, self.negOnes, Rt[hh], [Rtb[hh], self.cbuf], start=False, stop=True, sgc=True)
                if k + 1 < len(steps):
                    for hh in range(G_):
                        mm1(k + 1, hh)
                for hh in range(G_):
                    bank = hh * 2 + (k % 2)
                    si = hh * 2 + (k % 2)
                    S.op("act", lambda e, si=si, bank=bank: e.activation(Wt[si], self.ps[bank][:], AF.Exp),
                         reads=[self.psb[bank]], writes=[Wtb[si]])
                    if m >= 0:
                        S.op("dve", lambda e, si=si, m=m: e.tensor_tensor(Wt[si], Wt[si], self.sbmask[:, m, :], ALU.mult),
                             reads=[Wtb[si], self.cbuf], writes=[Wtb[si]])
                for hh in range(G_):
                    si = hh * 2 + (k % 2)
                    sl = sls[hh]
                    self.mm(4 + hh, sl["V"][:, sb, :], Wt[si], [sl["Vb"], Wtb[si]],
                            start=(k == 0), stop=(k == len(steps) - 1))
                for hh in range(G_):
                    si = hh * 2 + (k % 2)
                    if k == 0:
                        S.op("dve", lambda e, hh=hh, si=si: e.tensor_copy(Rt[hh], SPt[si]),
                             reads=[SPb[si]], writes=[Rtb[hh]])
                    elif k + 1 < len(steps):
                        S.op("dve", lambda e, hh=hh, si=si: e.tensor_tensor(Rt[hh], Rt[hh], SPt[si], ALU.add),
                             reads=[SPb[si], Rtb[hh]], writes=[Rtb[hh]])
            for hh in range(G_):
                sl = sls[hh]
                S.op("dve", lambda e, hh=hh, dst=sl["A"][:, t0:t0 + T]: e.tensor_copy(dst, self.ps[4 + hh][:]),
                     reads=[self.psb[4 + hh]], writes=[sl["Ab"]])
        for hh in range(G_):
            h = g * G_ + hh
            sl = sls[hh]
            S.dma(self.ac[h], sl["A"], reads=[sl["Ab"]], writes=[cb["a"][0]])
    self.full_barrier()
    if "sb2" in dbg:
        return
    Y = c(W0, [128, 16, 512], F32)
    AT = c(W0 + 32768, [128, 16, 512], BF16)
    self.small_layout(49152)
    Yb, ATb = Buf("Y"), Buf("AT")
    for j in range(NT):
        t0 = j * T
        S.dma(AT, self.ac[:, :, t0:t0 + T].rearrange("h d t -> d h t"), reads=[cb["a"][0]], writes=[ATb])
        self.out_proj("bwout", i_sb, 4, 512, 16, AT, ATb, Y, Yb)
        self.resid_back(Y, Yb, 7, lv[:, 2, :], xsrc, xsrc_buf, xdst, xdst_buf, t0)


Builder.emit_sb_pass = _emit_sb_pass


def _emit_dsa_pass(self, l, xsrc, xsrc_buf, xdst, xdst_buf):
    S = self.S
    cfg = self.cfg
    c = self.carve
    W0 = self.WORK
    ia = l // 2
    lv = self.lvec
    NT, NB, SL = cfg.NT, cfg.NB, cfg.S
    dbg = getattr(cfg, "dbg", "")
    qscale = float(128 ** -0.5)
    wiscale = float(16 ** -0.5 * 128 ** -0.5)
    CKVT = c(W0, [128, 2, SL], BF16) if SL == 4096 else c(W0, [128, 2, SL], BF16)
    CKV = c(W0 + 16384, [128, NB, 256], BF16)
    KIT = c(W0 + 32768, [128, SL], BF16)
    CKVTb, CKVb, KITb = Buf("CKVT"), Buf("CKV"), Buf("KIT")
    R0 = W0 + 40960
    R1 = R0 + 32768
    R2 = R1 + 32768
    R3 = R2 + 16384
    R4 = R3 + 5120
    QL = c(R3, [128, 4, 512], BF16)
    WI = c(R3 + 4096, [128, 4, 16], F32)
    QLb, WIb = Buf("QL"), Buf("WI")
    H = c(R2, [128, 16, 512], BF16)
    Hb = Buf("H")
    QLf = c(R0, [128, 4, 512], F32)
    CKf = c(R0 + 8192, [128, 2, 512], F32)
    KIf = c(R0 + 12288, [128, 512], F32)
    KIc = c(R0 + 14336, [128, 512], F32)
    KIb = c(R0 + 16384, [128, 512], BF16)
    QLfb, CKfb, KIfb, KIcb, KIbb = Buf("QLf"), Buf("CKf"), Buf("KIf"), Buf("KIc"), Buf("KIb")
    QIT = c(R0, [128, 16, 512], BF16)
    SC = c(R0 + 16384, [128, SL], F32)
    QITb, SCb = Buf("QIT"), Buf("SC")
    AT = c(R0, [128, 32, 512], BF16)
    ATb = Buf("AT")
    MT = c(R1, [128, NB, 512], BF16)
    MTb = Buf("MT")
    Y = c(R1, [128, 16, 512], F32)
    Yb = Buf("Y")
    MK = [c(R2 + i * 8192, [128, SL], BF16) for i in range(2)]
    MKb = [Buf("MK%d" % i) for i in range(2)]
    QA = c(R2, [128, 16, 512], BF16)
    QAb = Buf("QA")
    Wt = [c(R4 + i * 1024, [128, 512], BF16) for i in range(4)]
    Wtb = [Buf("Wt%d" % i) for i in range(4)]
    RB = c(R4 + 4096, [128, 8, 3, 128], BF16)
    RBb = Buf("RB")
    QH = [c(R4 + 10240 + i * 1024, [128, 512], BF16) for i in range(2)]
    QHb = [Buf("QH%d" % i) for i in range(2)]
    OL = c(R4 + 12288, [128, 2, 512], BF16)
    OLb = Buf("OL")
    RCP = c(R4 + 14336, [128, 512], F32)
    RCPb = Buf("RCP")
    M8 = c(R4 + 16384, [128, 8], F32)
    M8b = Buf("M8")
    RLf = [c(R4 + i * 2048, [128, 512], F32) for i in range(2)]
    RLfb = [Buf("RLf%d" % i) for i in range(2)]
    asm = self.asmall
    qn = asm[:, ia * 8:ia * 8 + 4]
    kvn = asm[:, ia * 8 + 4:ia * 8 + 6]
    ikn = asm[:, ia * 8 + 6:ia * 8 + 7]
    psT = [self.ps[b][:].bitcast(BF16) for b in range(8)]

    def evac(k, dst, src, bank, wb, scale=None):
        if k % 2 == 0:
            if scale is None:
                S.op("act", lambda e: e.copy(dst, src), reads=[self.psb[bank]], writes=wb)
            else:
                S.op("act", lambda e: e.activation(dst, src, AF.Identity, scale=scale), reads=[self.psb[bank]], writes=wb)
        else:
            if scale is None:
                S.op("dve", lambda e: e.tensor_copy(dst, src), reads=[self.psb[bank]], writes=wb)
            else:
                S.op("dve", lambda e: e.tensor_scalar(dst, src, scale, None, ALU.mult), reads=[self.psb[bank]], writes=wb)

    def rstd(bank):
        self.rstd_from_psum(bank)

    self.small_layout(R4 - W0)
    SQ, RS = self.SQ, self.RS
    SQb, RSb = self.SQb, self.RSb
    for j in range(NT):
        t0 = j * T
        nsb = 4 * j + 4
        self.norm_front(xsrc, xsrc_buf, t0, lv[:, 0, :], lv[:, 1, :], H, Hb)
        Wv, Wb = self.wnext(self.wb["awin"][ia, 0], self.wb_buf["awin"][ia], [16, 512])
        if not S.dry:
            for oc in range(4):
                bank = self.rot("ps", 4)
                for kc in range(16):
                    self.mm(bank, Wv[:, kc, oc * 128:(oc + 1) * 128], H[:, kc, :], [Wb, Hb],
                            start=(kc == 0), stop=(kc == 15), inc=(kc == 15))
                S.op("dve", lambda e, d_=QLf[:, oc, :], s_=self.ps[bank][:]: e.tensor_copy(d_, s_),
                     reads=[self.psb[bank]], writes=[QLfb])
                q = self.rot("sq", 3)
                S.op("act", lambda e, d_=SQ[q], s_=QLf[:, oc, :]: e.activation(d_, s_, AF.Square),
                     reads=[QLfb], writes=[SQb[q]])
                self.mm(7, self.ones512, SQ[q], [SQb[q], self.cbuf], start=(oc == 0), stop=(oc == 3))
            rstd(7)
            for oc in range(4):
                S.op("dve", lambda e, d_=QL[:, oc, :], s_=QLf[:, oc, :], g_=qn[:, oc:oc + 1]: e.scalar_tensor_tensor(
                    d_, s_, g_, RS, ALU.mult, ALU.mult), reads=[QLfb, RSb, self.cbuf], writes=[QLb])
        Wv, Wb = self.wnext(self.wb["awin"][ia, 1], self.wb_buf["awin"][ia], [16, 512])
        if not S.dry:
            for oc in range(2):
                bank = self.rot("ps", 4)
                for kc in range(16):
                    self.mm(bank, Wv[:, kc, oc * 128:(oc + 1) * 128], H[:, kc, :], [Wb, Hb],
                            start=(kc == 0), stop=(kc == 15), inc=(kc == 15))
                S.op("dve", lambda e, d_=CKf[:, oc, :], s_=self.ps[bank][:]: e.tensor_copy(d_, s_),
                     reads=[self.psb[bank]], writes=[CKfb])
                q = self.rot("sq", 3)
                S.op("act", lambda e, d_=SQ[q], s_=CKf[:, oc, :]: e.activation(d_, s_, AF.Square),
                     reads=[CKfb], writes=[SQb[q]])
                self.mm(7, self.ones256, SQ[q], [SQb[q], self.cbuf], start=(oc == 0), stop=(oc == 1))
            rstd(7)
            for oc in range(2):
                S.op("dve", lambda e, d_=CKVT[:, oc, t0:t0 + T], s_=CKf[:, oc, :], g_=kvn[:, oc:oc + 1]: e.scalar_tensor_tensor(
                    d_, s_, g_, RS, ALU.mult, ALU.mult), reads=[CKfb, RSb, self.cbuf], writes=[CKVTb])
            bank = self.rot("ps", 4)
            for kc in range(16):
                self.mm(bank, Wv[:, kc, 256:384], H[:, kc, :], [Wb, Hb], start=(kc == 0), stop=(kc == 15), inc=(kc == 15))
            S.op("dve", lambda e, s_=self.ps[bank][:]: e.tensor_copy(KIf, s_), reads=[self.psb[bank]], writes=[KIfb])
            S.op("act", lambda e: e.copy(KIb, KIf), reads=[KIfb], writes=[KIbb])
            self.mm(7, self.ones128, KIb, [KIbb, self.cbuf], start=True, stop=True)
            S.op("dve", lambda e, s_=self.ps[7][:]: e.tensor_tensor(KIc, KIf, s_, ALU.subtract),
                 reads=[KIfb, self.psb[7]], writes=[KIcb])
            q = self.rot("sq", 3)
            S.op("act", lambda e, d_=SQ[q]: e.activation(d_, KIc, AF.Square), reads=[KIcb], writes=[SQb[q]])
            self.mm(7, self.ones128, SQ[q], [SQb[q], self.cbuf], start=True, stop=True)
            rstd(7)
            S.op("dve", lambda e, d_=KIT[:, t0:t0 + T]: e.scalar_tensor_tensor(d_, KIc, ikn, RS, ALU.mult, ALU.mult),
                 reads=[KIcb, RSb, self.cbuf], writes=[KITb])
            for tb in range(4):
                bank = self.rot("ps", 4)
                for kc in range(16):
                    self.mm(bank, H[:, kc, tb * 128:(tb + 1) * 128], Wv[:, kc, 384:400], [Wb, Hb],
                            start=(kc == 0), stop=(kc == 15), inc=(kc == 15), out=self.ps[bank][:, 0:16])
                S.op("act", lambda e, d_=WI[:, tb, :], s_=self.ps[bank][:, 0:16]: e.activation(d_, s_, AF.Identity, scale=wiscale),
                     reads=[self.psb[bank]], writes=[WIb])
            for tb in range(4):
                bank = self.rot("ps", 4)
                for cc in range(2):
                    self.S.op("pe", lambda e, o_=psT[bank][:, cc * 128:(cc + 1) * 128],
                              i_=CKVT[:, cc, t0 + tb * 128:t0 + (tb + 1) * 128]: e.transpose(o_, i_, self.ident),
                              reads=[CKVTb, self.cbuf], writes=[self.psb[bank]], inc=(cc == 1))
                S.op("dve", lambda e, d_=CKV[:, 4 * j + tb, :], s_=psT[bank][:, 0:256]: e.tensor_copy(d_, s_),
                     reads=[self.psb[bank]], writes=[CKVb])
        self.full_barrier()
        Wv, Wb = self.wnext(self.wb["awiqb"][ia, 0], self.wb_buf["awiqb"][ia], [4, 2048])
        if not S.dry:
            for hi in range(16):
                bank = self.rot("ps", 4)
                for kc in range(4):
                    self.mm(bank, Wv[:, kc, hi * 128:(hi + 1) * 128], QL[:, kc, :], [Wb, QLb],
                            start=(kc == 0), stop=(kc == 3), inc=(kc == 3))
                evac(hi, QIT[:, hi, :], self.ps[bank][:], bank, [QITb])
        if not S.dry:
            for m in range(1, 4):
                S.op("dve", lambda e, d_=MT[:, 4 * j + m, 0:128 * m]: e.memset(d_, 0.0), writes=[MTb])
            for qb in range(4):
                nk = (4 * j + qb + 1) * 128
                for r in range(j + 1):
                    w = min(512, nk - r * 512)
                    for hi in range(16):
                        bank = self.rot("ps", 4)
                        self.mm(bank, QIT[:, hi, qb * 128:(qb + 1) * 128], KIT[:, r * 512:r * 512 + w], [QITb, KITb],
                                start=True, stop=True, out=self.ps[bank][:, 0:w])
                        ri = self.rot("rl", 2)
                        S.op("act", lambda e, d_=RLf[ri][:, 0:w], s_=self.ps[bank][:, 0:w]: e.activation(d_, s_, AF.Relu),
                             reads=[self.psb[bank]], writes=[RLfb[ri]])
                        scs = SC[:, r * 512:r * 512 + w]
                        wcol = WI[:, qb, hi:hi + 1]
                        if hi == 0:
                            S.op("dve", lambda e, d_=scs, s_=RLf[ri][:, 0:w], w_=wcol: e.tensor_scalar(d_, s_, w_, None, ALU.mult),
                                 reads=[RLfb[ri], WIb], writes=[SCb])
                        else:
                            S.op("dve", lambda e, d_=scs, s_=RLf[ri][:, 0:w], w_=wcol: e.scalar_tensor_tensor(
                                d_, s_, w_, d_, ALU.mult, ALU.add), reads=[RLfb[ri], WIb, SCb], writes=[SCb])
                dg = SC[:, nk - 128:nk]
                S.op("dve", lambda e, d_=dg: e.tensor_tensor(d_, d_, self.causneg, ALU.add), reads=[SCb, self.cbuf], writes=[SCb])
                scv = SC[:, 0:nk]
                for it in range(32):
                    S.op("dve", lambda e, s_=scv: e.max(M8, s_), reads=[SCb], writes=[M8b])
                    S.op("dve", lambda e, s_=scv: e.match_replace(s_, M8, s_, NEGFILL), reads=[SCb, M8b], writes=[SCb])
                S.op("dve", lambda e, d_=dg: e.tensor_tensor(d_, d_, self.causneg, ALU.add), reads=[SCb, self.cbuf], writes=[SCb])
                mk = MK[qb % 2]
                mkb = MKb[qb % 2]
                S.op("dve", lambda e, d_=mk[:, 0:nk], s_=scv: e.tensor_single_scalar(d_, s_, NEGFILL, ALU.is_equal),
                     reads=[SCb], writes=[mkb])
                nblk = 4 * j + qb + 1
                for s0 in range(0, nblk, 4):
                    n4 = min(4, nblk - s0)
                    bank = self.rot("ps", 4)
                    for i4 in range(n4):
                        sb = s0 + i4
                        self.S.op("pe", lambda e, o_=psT[bank][:, i4 * 128:(i4 + 1) * 128], i_=mk[:, sb * 128:(sb + 1) * 128]:
                                  e.transpose(o_, i_, self.ident), reads=[mkb, self.cbuf], writes=[self.psb[bank]], inc=(i4 == n4 - 1))
                    src = psT[bank][:, 0:n4 * 128].rearrange("p (a b) -> p a b", a=n4)
                    evac(s0 // 4, MT[:, s0:s0 + n4, qb * 128:(qb + 1) * 128], src, bank, [MTb])
        self.full_barrier()
        for hg in range(4):
            Wg, Wqb = self.wnext(self.wb["awg"][ia, hg], self.wb_buf["awg"][ia], [8192])
            if S.dry:
                continue
            Wkb = Wub = Wqb
            Wq = Wg[:, 0:4096].rearrange("p (k n) -> p k n", k=4)
            Wk = Wg[:, 4096:6144].rearrange("p (h c) -> p h c", h=8)
            Wu = Wg[:, 6144:8192].rearrange("p (h c v) -> p h c v", h=8, c=2)
            S.dma(RB.rearrange("p a b c -> p (a b c)"), self.d["rbt"][:, hg, :], writes=[RBb], queue="pool")
            for hh in range(8):
                bank = self.rot("ps", 2)
                for kc in range(4):
                    self.mm(bank, Wq[:, kc, hh * 128:(hh + 1) * 128], QL[:, kc, :], [Wqb, QLb],
                            start=(kc == 0), stop=(kc == 3), inc=(kc == 3))
                qi = hh % 2
                S.op("act", lambda e, d_=QH[qi], s_=self.ps[bank][:]: e.activation(d_, s_, AF.Identity, scale=qscale),
                     reads=[self.psb[bank]], writes=[QHb[qi]])
                for cc in range(2):
                    bank2 = self.rot("ps", 2)
                    self.mm(bank2, Wk[:, hh, cc * 128:(cc + 1) * 128], QH[qi], [Wkb, QHb[qi]], start=True, stop=True)
                    S.op("dve", lambda e, d_=QA[:, hh * 2 + cc, :], s_=self.ps[bank2][:]: e.tensor_copy(d_, s_),
                         reads=[self.psb[bank2]], writes=[QAb])
            for hh in range(8):
                h = hg * 8 + hh
                ob = 2 + 3 * (hh % 2)

                def Lmm(sb, lb):
                    for cc in range(2):
                        self.mm(lb, CKVT[:, cc, sb * 128:(sb + 1) * 128], QA[:, hh * 2 + cc, :], [CKVTb, QAb],
                                start=(cc == 0), stop=(cc == 1), inc=(cc == 1))
                    m = sb - 4 * j
                    if m >= -1:
                        for qb in range(4):
                            rel = qb - m
                            if rel < 0:
                                continue
                            pl = min(rel, 2)
                            self.mm(lb, self.ident, RB[:, hh, pl, :], [RBb, self.cbuf], start=False, stop=True,
                                    out=self.ps[lb][:, qb * 128:(qb + 1) * 128], sgc=True)

                Lmm(0, 0)
                for sb in range(nsb):
                    lb = sb % 2
                    if sb + 1 < nsb:
                        Lmm(sb + 1, (sb + 1) % 2)
                    wi = self.rot("xc", 4)
                    near = (sb - 4 * j) >= -1
                    if near:
                        S.op("act", lambda e, d_=Wt[wi], s_=self.ps[lb][:]: e.activation(d_, s_, AF.Exp),
                             reads=[self.psb[lb]], writes=[Wtb[wi]])
                    else:
                        S.op("act", lambda e, d_=Wt[wi], s_=self.ps[lb][:], b_=self.cbias[:, h:h + 1]: e.activation(d_, s_, AF.Exp, bias=b_),
                             reads=[self.psb[lb], self.cbuf], writes=[Wtb[wi]])
                    S.op("dve", lambda e, d_=Wt[wi], m_=MT[:, sb, :]: e.tensor_tensor(d_, d_, m_, ALU.mult),
                         reads=[Wtb[wi], MTb], writes=[Wtb[wi]])
                    for cc in range(2):
                        self.mm(ob + cc, CKV[:, sb, cc * 128:(cc + 1) * 128], Wt[wi], [CKVb, Wtb[wi]],
                                start=(sb == 0), stop=(sb == nsb - 1), inc=False)
                    self.mm(ob + 2, self.ones1, Wt[wi], [self.cbuf, Wtb[wi]], start=(sb == 0), stop=(sb == nsb - 1))
                S.op("dve", lambda e, s_=self.ps[ob + 2][:]: e.reciprocal(RCP, s_), reads=[self.psb[ob + 2]], writes=[RCPb])
                for cc in range(2):
                    S.op("dve", lambda e, d_=OL[:, cc, :], s_=self.ps[ob + cc][:]: e.tensor_tensor(d_, s_, RCP, ALU.mult),
                         reads=[self.psb[ob + cc], RCPb], writes=[OLb])
                bank = self.rot("ps", 2)
                for cc in range(2):
                    self.mm(bank, Wu[:, hh, cc, :], OL[:, cc, :], [Wub, OLb], start=(cc == 0), stop=(cc == 1), inc=(cc == 1))
                S.op("act", lambda e, d_=AT[:, h, :], s_=self.ps[bank][:]: e.copy(d_, s_), reads=[self.psb[bank]], writes=[ATb])
        self.full_barrier()
        self.out_proj("awout", ia, 8, 256, 32, AT, ATb, Y, Yb)
        self.resid_back(Y, Yb, 7, lv[:, 2, :], xsrc, xsrc_buf, xdst, xdst_buf, t0)
        self.full_barrier()


Builder.emit_dsa_pass = _emit_dsa_pass
```

```python
import numpy as np
import ml_dtypes
from contextlib import ExitStack
import concourse.bass as bass
import concourse.mybir as mybir
from concourse.bass_utils import run_bass_kernel_spmd

F32 = mybir.dt.float32
BF16 = mybir.dt.bfloat16
ALU = mybir.AluOpType
AF = mybir.ActivationFunctionType
NPBF = ml_dtypes.bfloat16

D = 2048
DC = 16
DFF = 8192
T = 512
NEGFILL = -1.0e30
CAUSFILL = -2.0e30
EPS = 1e-6


class Buf:
    __slots__ = ("name", "w", "r", "psum")

    def __init__(self, name="", psum=False):
        self.name = name
        self.w = None
        self.r = {}
        self.psum = psum


class Sched:
    ENGS = ("pe", "act", "dve", "pool", "sp")

    def __init__(self, nc, stack, n_dma_sems=32, n_pool_sems=16):
        self.nc = nc
        self.dry = False
        self.prog = {e: [] for e in self.ENGS}
        self.sems = {}
        self.count = {}
        for e in ("pe", "act", "dve", "pool"):
            self.sems[e] = stack.enter_context(nc.semaphore("c_" + e))
            self.count[e] = 0
        self.ndma = {"sp": n_dma_sems, "pool": n_pool_sems}
        for q, n in self.ndma.items():
            for i in range(n):
                k = "%s%d" % (q, i)
                self.sems[k] = stack.enter_context(nc.semaphore(k))
                self.count[k] = 0
        self.dma_rr = {"sp": 0, "pool": 0}
        self.seen = {e: {} for e in self.ENGS}
        self.ninst = {e: 0 for e in self.ENGS}

    def _wait(self, eng, key, val):
        if val <= 0 or self.seen[eng].get(key, 0) >= val:
            return
        self.seen[eng][key] = val
        self.prog[eng].append(("wait", key, val))

    def _deps(self, eng, reads, writes, skip_self=False):
        deps = {}
        for b in reads:
            if b.w is not None:
                k, v = b.w
                if v > deps.get(k, 0):
                    deps[k] = v
            if b.psum:
                for k, v in b.r.items():
                    if k != eng and v > deps.get(k, 0):
                        deps[k] = v
        for b in writes:
            if b.w is not None:
                k, v = b.w
                if v > deps.get(k, 0):
                    deps[k] = v
            for k, v in b.r.items():
                if v > deps.get(k, 0):
                    deps[k] = v
        for k, v in deps.items():
            if skip_self and k == eng:
                continue
            self._wait(eng, k, v)

    def op(self, eng, fn, reads=(), writes=(), inc=True):
        if self.dry:
            return
        self._deps(eng, reads, writes, skip_self=(eng == "pe"))
        if inc:
            self.count[eng] += 1
            tok = (eng, self.count[eng])
            self.prog[eng].append(("inst", fn, eng, 1))
        else:
            tok = (eng, self.count[eng] + 1)
            self.prog[eng].append(("inst", fn, None, 0))
        self.ninst[eng] += 1
        for b in reads:
            if b.r.get(tok[0], 0) < tok[1]:
                b.r[tok[0]] = tok[1]
        for b in writes:
            b.w = tok
            b.r = {}

    def dma(self, out_ap, in_ap, reads=(), writes=(), queue="sp"):
        if self.dry:
            return
        i = self.dma_rr[queue]
        self.dma_rr[queue] = (i + 1) % self.ndma[queue]
        key = "%s%d" % (queue, i)
        self._wait(queue, key, self.count[key])
        self._deps(queue, reads, writes)
        self.count[key] += 16
        v = self.count[key]
        self.prog[queue].append(
            ("inst", (lambda e, o=out_ap, a=in_ap: e.dma_start(out=o, in_=a)), key, 16))
        self.ninst[queue] += 1
        for b in reads:
            b.r[key] = v
        for b in writes:
            b.w = (key, v)
            b.r = {}

    def barrier(self, engs=("pe", "act", "dve", "pool")):
        if self.dry:
            return
        for e in engs:
            for f in ("pe", "act", "dve", "pool"):
                if f != e:
                    self._wait(e, f, self.count[f])

    def wait_all(self, eng, bufs):
        if self.dry:
            return
        self._deps(eng, (), bufs)

    def replay(self):
        nc = self.nc
        engobj = {"pe": "tensor", "act": "scalar", "dve": "vector", "pool": "gpsimd", "sp": "sync"}
        with nc.Block() as block:
            for e in self.ENGS:
                prog = self.prog[e]
                if not prog:
                    continue

                def body(eng, prog=prog):
                    for item in prog:
                        if item[0] == "wait":
                            eng.wait_ge(self.sems[item[1]], item[2])
                        else:
                            ins = item[1](eng)
                            if item[2] is not None:
                                ins.then_inc(self.sems[item[2]], item[3])
                getattr(block, engobj[e])(body)


def blockify(W, ncols):
    K, N = W.shape
    return np.ascontiguousarray(
        W.reshape(K // 128, 128, N // ncols, ncols).transpose(2, 1, 0, 3))


def vec_pk(v):
    return np.ascontiguousarray(v.reshape(-1, 128).T)


def t5_bucket_np(dist):
    dist = np.asarray(dist)
    d = np.maximum(dist, 1).astype(np.float32)
    large = 16 + (np.log(d / 16) / np.float32(np.log(128 / 16)) * 16).astype(np.int32)
    large = np.minimum(large, 31)
    return np.where(dist < 16, dist, large)


class Cfg:
    def __init__(self, S_len=4096, layers=(0, 1, 2, 3), do_mixer=True, do_mlp=True):
        self.S = S_len
        self.NT = S_len // T
        self.NB = S_len // 128
        self.layers = tuple(layers)
        self.do_mixer = do_mixer
        self.do_mlp = do_mlp


def host_consts():
    c = {}
    c["ident"] = np.eye(128, dtype=np.float32).astype(NPBF)
    cm = np.zeros((128, 8, 128), np.float32)
    cm[:, 0, :] = 1.0
    cm[:, 1, :] = 1.0 / 2048
    cm[:, 2, :] = 1.0 / 512
    cm[:, 3, :] = 1.0 / 256
    cm[:, 4, :] = 1.0 / 128
    j = np.arange(128)[:, None]
    s = np.arange(128)[None, :]
    cm[:, 5, :] = -(j >= s).astype(np.float32)
    cm[:, 6, :] = -1.0
    c["cmats"] = cm.astype(NPBF)
    tt = np.arange(512)[None, None, :]
    mm = np.arange(4)[None, :, None]
    ss = np.arange(128)[:, None, None]
    c["sbmask"] = ((tt - 128 * mm) > ss).astype(np.float32).astype(NPBF)
    c["causneg"] = np.where(s > j, CAUSFILL, 0.0).astype(np.float32)
    c["pow2"] = np.ascontiguousarray(np.broadcast_to((2.0 ** -np.arange(32))[None, :], (128, 32))).astype(np.float32)
    return c


def host_prepare(inputs, cfg):
    f32 = np.float32
    sh = {}
    sh.update(host_consts())
    L = 4
    ada_w = inputs["ada_w"]
    sh["adaw"] = np.stack([blockify(ada_w[l], 512) for l in range(L)])
    sh["adab"] = np.ascontiguousarray(
        inputs["ada_b"].reshape(L, 96, 128).transpose(2, 0, 1).reshape(128, L * 96))
    sh["normg"] = np.ascontiguousarray(
        inputs["norm_g"].reshape(L, 4, 16, 128).transpose(3, 0, 1, 2).reshape(128, L * 64))
    sh["w1"] = np.stack([blockify(inputs["mlp_w1"][l], 512) for l in range(L)])
    sh["w2"] = np.stack([blockify(inputs["mlp_w2"][l], 128) for l in range(L)])
    sh["bwin"] = np.stack([blockify(inputs["b_w_in"][i], 512) for i in range(2)])
    sh["bwout"] = np.stack([blockify(inputs["b_w_out"][i], 512) for i in range(2)])
    awin = np.zeros((2, 2048, 1024), f32)
    awin[:, :, :912] = inputs["a_w_in"]
    sh["awin"] = np.stack([blockify(awin[i], 512) for i in range(2)])
    sh["awqb"] = np.stack([blockify(inputs["a_w_qb"][i], 1024) for i in range(2)])
    sh["awiqb"] = np.stack([blockify(inputs["a_w_idx_qb"][i], 2048) for i in range(2)])
    wuk = inputs["a_w_uk"]
    sh["awuk"] = np.ascontiguousarray(
        wuk.reshape(2, 4, 8, 256, 128).transpose(0, 1, 4, 2, 3))
    wuv = inputs["a_w_uv"]
    sh["awuv"] = np.ascontiguousarray(
        wuv.reshape(2, 4, 8, 2, 128, 128).transpose(0, 1, 4, 2, 3, 5))
    sh["awg"] = np.ascontiguousarray(np.concatenate(
        [sh.pop("awqb").reshape(2, 4, 128, 4096), sh.pop("awuk").reshape(2, 4, 128, 2048),
         sh.pop("awuv").reshape(2, 4, 128, 2048)], axis=3))
    sh["awout"] = np.stack([blockify(inputs["a_w_out"][i], 256) for i in range(2)])
    sm = np.zeros((128, 2, 8), f32)
    for i in range(2):
        sm[:, i, 0:4] = vec_pk(inputs["a_q_norm"][i])
        sm[:, i, 4:6] = vec_pk(inputs["a_kv_norm"][i])
        sm[:, i, 6:7] = vec_pk(inputs["a_idx_k_norm"][i])
    sh["asmall"] = sm.reshape(128, 16)
    rb = inputs["rel_bias"]
    s_ = np.arange(128)[:, None]
    t_ = np.arange(128)[None, :]
    b0 = t5_bucket_np(np.maximum(t_ - s_, 0))
    b1 = t5_bucket_np(t_ - s_ + 128)
    rbt = np.zeros((128, 32, 3, 128), f32)
    rbt[:, :, 0, :] = rb[b0].transpose(0, 2, 1)
    rbt[:, :, 1, :] = rb[b1].transpose(0, 2, 1)
    rbt[:, :, 2, :] = rb[31][None, :, None]
    sh["rbt"] = np.ascontiguousarray(rbt.reshape(128, 4, 8 * 3 * 128))
    sh["cbias"] = np.ascontiguousarray(np.broadcast_to(rb[31][None, :], (128, 32))).astype(f32)
    percore = []
    for b in range(inputs["x"].shape[0]):
        pc = {}
        pc["xT"] = np.ascontiguousarray(inputs["x"][b, :cfg.S].T)
        pc["cT"] = vec_pk(inputs["c"][b])
        percore.append(pc)
    return sh, percore


class Builder:
    def __init__(self, cfg):
        self.cfg = cfg
        self.nc = bass.Bass("TRN2", target_bir_lowering=False)
        self.stack = ExitStack()
        self.S = Sched(self.nc, self.stack)
        self.wplan = []
        self.wpos = 0
        self.wissued = 0

    def din(self, name, shape, dt=F32):
        return self.nc.dram_tensor(name, list(shape), dt, kind="ExternalInput").ap()

    def dscr(self, name, shape, dt):
        return self.nc.dram_tensor(name, list(shape), dt, kind="Internal").ap()

    def declare(self):
        cfg = self.cfg
        S_ = cfg.S
        d = {}
        d["xT"] = self.din("xT", [D, S_])
        d["cT"] = self.din("cT", [128, 16])
        d["ident"] = self.din("ident", [128, 128], BF16)
        d["cmats"] = self.din("cmats", [128, 8, 128], BF16)
        d["sbmask"] = self.din("sbmask", [128, 4, 512], BF16)
        d["causneg"] = self.din("causneg", [128, 128])
        d["pow2"] = self.din("pow2", [128, 32])
        d["adaw"] = self.din("adaw", [4, 24, 128, 16, 512])
        d["adab"] = self.din("adab", [128, 384])
        d["normg"] = self.din("normg", [128, 256])
        d["w1"] = self.din("w1", [4, 16, 128, 16, 512])
        d["w2"] = self.din("w2", [4, 16, 128, 64, 128])
        d["bwin"] = self.din("bwin", [2, 12, 128, 16, 512])
        d["bwout"] = self.din("bwout", [2, 4, 128, 16, 512])
        d["awin"] = self.din("awin", [2, 2, 128, 16, 512])
        d["awg"] = self.din("awg", [2, 4, 128, 8192])
        d["awiqb"] = self.din("awiqb", [2, 1, 128, 4, 2048])
        d["awout"] = self.din("awout", [2, 8, 128, 32, 256])
        d["asmall"] = self.din("asmall", [128, 16])
        d["rbt"] = self.din("rbt", [128, 4, 8 * 3 * 128])
        d["cbias"] = self.din("cbias", [128, 32])
        self.d = d
        self.yT = self.nc.dram_tensor("yT", [D, S_], F32, kind="ExternalOutput").ap()
        self.xs = self.dscr("xs", [D, S_], F32)
        mk = lambda nm: [[Buf("%s_%d_%d" % (nm, j, ch)) for ch in range(DC)] for j in range(cfg.NT)]
        self.xs_buf = mk("xs")
        self.xin_buf = mk("xin")
        self.yT_buf = mk("yT")
        self.wb = {}
        self.wb_buf = {}
        for name in ("w1", "w2", "bwin", "bwout", "awin", "awg", "awiqb", "awout"):
            shp = list(d[name].shape)
            self.wb[name] = self.dscr(name + "_bf", shp, BF16)
            self.wb_buf[name] = [[] for i in range(shp[0])]
        self.qc = self.dscr("qcache", [16, 128, S_], BF16)
        self.kc = self.dscr("kcache", [16, 128, S_], BF16)
        self.vc = self.dscr("vcache", [16, 128, cfg.NB, 128], BF16)
        self.ac = self.dscr("acache", [16, 128, S_], BF16)
        self.qc_buf, self.kc_buf, self.vc_buf, self.ac_buf = Buf("qc"), Buf("kc"), Buf("vc"), Buf("ac")

    def alloc(self):
        nc = self.nc
        self.ARENA_BYTES = 207 * 1024
        self.arena = self.stack.enter_context(nc.sbuf_tensor("arena", [128, self.ARENA_BYTES // 2], BF16))
        self.ps = []
        self.psb = []
        for i in range(8):
            self.ps.append(self.stack.enter_context(nc.psum_tensor("ps%d" % i, [128, 512], F32)))
            self.psb.append(Buf("ps%d" % i, psum=True))

    def carve(self, off_bytes, shape, dt):
        esz = 4 if dt == F32 else 2
        n = int(np.prod(shape[1:])) * esz
        assert off_bytes % 4 == 0 and off_bytes + n <= self.ARENA_BYTES, (off_bytes, n)
        a = self.arena[:, off_bytes // 2:(off_bytes + n) // 2]
        if dt == F32:
            a = a.bitcast(F32)
        if len(shape) == 3:
            a = a.rearrange("p (a b) -> p a b", a=shape[1])
        elif len(shape) == 4:
            a = a.rearrange("p (a b c) -> p a b c", a=shape[1], b=shape[2])
        return a

    def wnext(self, dram_ap, buf, shape, queue="sp"):
        if self.S.dry:
            self.wplan.append((dram_ap, buf, shape, queue))
            return None, None
        n = self.wpos
        self.wpos += 1
        while self.wissued < min(len(self.wplan), n + self.WDEPTH):
            i = self.wissued
            ap_i, buf_i, shp_i, q_i = self.wplan[i]
            slot = i % self.NSLOT
            view = self.carve(self.RING_OFF + slot * 16384, [128] + list(shp_i), BF16)
            self.S.dma(view, ap_i, reads=list(buf_i), writes=[self.ring_buf[slot]], queue=q_i)
            self.wissued += 1
        slot = n % self.NSLOT
        shp = self.wplan[n][2]
        return self.carve(self.RING_OFF + slot * 16384, [128] + list(shp), BF16), self.ring_buf[slot]

    def mm(self, ps_i, lhsT, rhs, rd, start, stop, inc=None, out=None, sgc=False):
        o = self.ps[ps_i][:] if out is None else out
        if inc is None:
            inc = True
        if sgc:
            self.S.op("pe", lambda e: e.matmul(o, lhsT, rhs, start=start, stop=stop, skip_group_check=True),
                      reads=rd, writes=[self.psb[ps_i]], inc=inc)
        else:
            self.S.op("pe", lambda e: e.matmul(o, lhsT, rhs, start=start, stop=stop),
                      reads=rd, writes=[self.psb[ps_i]], inc=inc)

    def cast_weights(self, name, idx):
        src = self.d[name][idx]
        dst = self.wb[name][idx]
        nd = len(src.shape)
        if nd == 4:
            a, b, c, e = src.shape
            src2 = src.rearrange("a b c e -> (a b) (c e)")
            dst2 = dst.rearrange("a b c e -> (a b) (c e)")
        elif nd == 5:
            src2 = src.rearrange("a b c e f -> (a b) (c e f)")
            dst2 = dst.rearrange("a b c e f -> (a b) (c e f)")
        else:
            src2 = src.rearrange("a b c -> (a b) c")
            dst2 = dst.rearrange("a b c -> (a b) c")
        rows = src2.shape[0]
        step = 128 * max(1, (8 << 20) // (128 * src2.shape[1] * 4))
        for r0 in range(0, rows, step):
            r1 = min(rows, r0 + step)
            bf = Buf("%s_%d_%d" % (name, idx, r0))
            self.wb_buf[name][idx].append(bf)
            self.S.dma(dst2[r0:r1], src2[r0:r1], writes=[bf], queue="pool")

    def setup_layout(self):
        c = self.carve
        self.ident = c(0, [128, 128], BF16)
        self.cmats = c(256, [128, 8, 128], BF16)
        self.ones1 = self.cmats[:, 0, :]
        self.onesD = self.cmats[:, 1, :]
        self.ones512 = self.cmats[:, 2, :]
        self.ones256 = self.cmats[:, 3, :]
        self.ones128 = self.cmats[:, 4, :]
        self.negU = self.cmats[:, 5, :]
        self.negOnes = self.cmats[:, 6, :]
        self.sbmask = c(2304, [128, 4, 512], BF16)
        self.causneg = c(6400, [128, 128], F32)
        self.cT = c(6912, [128, 16], F32)
        self.csil = c(6976, [128, 16], BF16)
        self.adab = c(7008, [128, 384], F32)
        self.normg = c(8544, [128, 256], F32)
        self.modv = c(9568, [128, 384], F32)
        self.lvec = c(11104, [128, 6, 16], F32)
        self.asmall = c(11488, [128, 16], F32)
        self.cbias = c(11552, [128, 32], F32)
        self.ltmp = c(11680, [128, 16], F32)
        self.pow2 = c(11744, [128, 32], F32)
        self.cbuf = Buf("consts")
        self.modv_buf = Buf("modv")
        self.lvec_buf = Buf("lvec")
        self.RING_OFF = 12288
        self.NSLOT = 3
        self.WDEPTH = 3
        self.ring_buf = [Buf("ring%d" % i) for i in range(self.NSLOT)]
        self.WORK = self.RING_OFF + self.NSLOT * 16384

    def emit_init(self):
        S = self.S
        d = self.d
        for dst, src in ((self.ident, d["ident"]), (self.cmats, d["cmats"]), (self.sbmask, d["sbmask"]),
                         (self.causneg, d["causneg"]), (self.cT, d["cT"]), (self.adab, d["adab"]),
                         (self.normg, d["normg"]), (self.asmall, d["asmall"]), (self.cbias, d["cbias"]),
                         (self.pow2, d["pow2"])):
            S.dma(dst, src, writes=[self.cbuf])
        S.op("act", lambda e: e.activation(self.csil, self.cT, AF.Silu), reads=[self.cbuf], writes=[self.cbuf])
        S.barrier()

    def emit_ada(self, l):
        S = self.S
        bank = 6
        for blk in range(24):
            Wv, Wb = self.wnext(self.d["adaw"][l, blk], [], [16, 512], queue="pool")
            if S.dry:
                continue
            for oc in range(4):
                col = blk * 4 + oc
                for kc in range(16):
                    self.mm(bank, Wv[:, kc, oc * 128:(oc + 1) * 128], self.csil[:, kc:kc + 1],
                            [Wb, self.cbuf], start=(kc == 0), stop=(kc == 15), inc=(kc == 15),
                            out=self.ps[bank][:, col:col + 1])
        S.op("dve", lambda e: e.tensor_tensor(self.modv[:, l * 96:(l + 1) * 96], self.ps[bank][:, 0:96],
                                              self.adab[:, l * 96:(l + 1) * 96], ALU.add),
             reads=[self.psb[bank], self.cbuf], writes=[self.modv_buf])

    def emit_layer_vecs(self, l):
        S = self.S
        mv = lambda m: self.modv[:, l * 96 + m * 16: l * 96 + (m + 1) * 16]
        g = lambda i: self.normg[:, (l * 4 + i) * 16:(l * 4 + i + 1) * 16]
        lv = self.lvec
        rd = [self.modv_buf, self.cbuf]
        wr = [self.lvec_buf]
        for (dst, sc, gi) in ((0, 1, 0), (3, 4, 2)):
            S.op("dve", lambda e, sc=sc: e.tensor_scalar(self.ltmp, mv(sc), 1.0, None, ALU.add), reads=rd, writes=wr)
            S.op("dve", lambda e, dst=dst, gi=gi: e.tensor_tensor(lv[:, dst, :], self.ltmp, g(gi), ALU.mult), reads=rd + wr, writes=wr)
        for (dst, sh) in ((1, 0), (4, 3)):
            S.op("dve", lambda e, dst=dst, sh=sh: e.tensor_copy(lv[:, dst, :], mv(sh)), reads=rd, writes=wr)
        for (dst, gt, gi) in ((2, 2, 1), (5, 5, 3)):
            S.op("dve", lambda e, dst=dst, gt=gt, gi=gi: e.tensor_tensor(lv[:, dst, :], mv(gt), g(gi), ALU.mult), reads=rd, writes=wr)

    def xview(self, ap, t0, n=T):
        return ap.rearrange("(c p) t -> p c t", p=128)[:, :, t0:t0 + n]

    def small_layout(self, base):
        c = self.carve
        o = self.WORK + base
        self.SQ = [c(o + i * 1024, [128, 512], BF16) for i in range(3)]
        self.SQb = [Buf("sq%d" % i) for i in range(3)]
        o += 3072
        self.RS = c(o, [128, 512], F32)
        self.RSb = Buf("rs")
        o += 2048
        self.RS2 = c(o, [128, 512], F32)
        self.RS2b = Buf("rs2")
        o += 2048
        self.T1 = [c(o + i * 2048, [128, 512], F32) for i in range(2)]
        self.T1b = [Buf("t1%d" % i) for i in range(2)]
        o += 4096
        self.XC = [c(o + i * 2048, [128, 512], F32) for i in range(4)]
        self.XCb = [Buf("xc%d" % i) for i in range(4)]
        o += 8192
        self.RL = [c(o + i * 1024, [128, 512], BF16) for i in range(2)]
        self.RLb = [Buf("rl%d" % i) for i in range(2)]
        o += 2048
        self.rr = {"sq": 0, "t1": 0, "xc": 0, "rl": 0, "ps": 0}
        return o - self.WORK

    def rot(self, key, n):
        i = self.rr[key]
        self.rr[key] = (i + 1) % n
        return i

    def rstd_from_psum(self, bank, which=0):
        S = self.S
        RS, RSb = (self.RS, self.RSb) if which == 0 else (self.RS2, self.RS2b)
        S.op("act", lambda e: e.activation(RS, self.ps[bank][:], AF.Ln, bias=EPS),
             reads=[self.psb[bank]], writes=[RSb])
        S.op("act", lambda e: e.activation(RS, RS, AF.Exp, scale=-0.5), reads=[RSb], writes=[RSb])

    def nf_chunk(self, xsrc, xsrc_buf, t0, ch):
        S = self.S
        XC, SQ = self.XC, self.SQ
        xv = self.xview(xsrc, t0)
        k = self.rot("xc", 4)
        S.dma(XC[k], xv[:, ch, :], reads=[xsrc_buf[t0 // T][ch]], writes=[self.XCb[k]])
        q = self.rot("sq", 3)
        S.op("act", lambda e, k=k, q=q: e.activation(SQ[q], XC[k], AF.Square),
             reads=[self.XCb[k]], writes=[self.SQb[q]])
        return q

    def nf_mm(self, q, ch, bank):
        self.mm(bank, self.onesD, self.SQ[q], [self.SQb[q], self.cbuf], start=(ch == 0), stop=(ch == DC - 1))

    def nf_pass2(self, xsrc, xsrc_buf, t0, avec, bvec, H, Hb, bank):
        S = self.S
        XC, T1, RS = self.XC, self.T1, self.RS2
        xv = self.xview(xsrc, t0)
        self.rstd_from_psum(bank, which=1)
        for ch in range(DC):
            k = self.rot("xc", 4)
            S.dma(XC[k], xv[:, ch, :], reads=[xsrc_buf[t0 // T][ch]], writes=[self.XCb[k]])
            i = self.rot("t1", 2)
            S.op("dve", lambda e, k=k, i=i, ch=ch: e.scalar_tensor_tensor(
                T1[i], XC[k], avec[:, ch:ch + 1], RS, ALU.mult, ALU.mult),
                reads=[self.XCb[k], self.RS2b, self.lvec_buf], writes=[self.T1b[i]])
            S.op("act", lambda e, i=i, ch=ch: e.activation(H[:, ch, :], T1[i], AF.Identity, bias=bvec[:, ch:ch + 1]),
                 reads=[self.T1b[i], self.lvec_buf], writes=[Hb])

    def norm_front(self, xsrc, xsrc_buf, t0, avec, bvec, H, Hb, bank=6):
        for ch in range(DC):
            q = self.nf_chunk(xsrc, xsrc_buf, t0, ch)
            self.nf_mm(q, ch, bank)
        self.nf_pass2(xsrc, xsrc_buf, t0, avec, bvec, H, Hb, bank)

    def resid_back(self, Y, Yb, bank, gvec, xsrc, xsrc_buf, xdst, xdst_buf, t0):
        S = self.S
        XC, SQ, T1, RL, RS = self.XC, self.SQ, self.T1, self.RL, self.RS
        self.rstd_from_psum(bank)
        xv = self.xview(xsrc, t0)
        ov = self.xview(xdst, t0)
        for ch in range(DC):
            k = self.rot("xc", 4)
            S.dma(XC[k], xv[:, ch, :], reads=[xsrc_buf[t0 // T][ch]], writes=[self.XCb[k]])
            i = self.rot("t1", 2)
            S.op("dve", lambda e, i=i, ch=ch: e.scalar_tensor_tensor(
                T1[i], Y[:, ch, :], gvec[:, ch:ch + 1], RS, ALU.mult, ALU.mult),
                reads=[Yb, self.RSb, self.lvec_buf], writes=[self.T1b[i]])
            S.op("dve", lambda e, i=i, k=k: e.tensor_tensor(XC[k], XC[k], T1[i], ALU.add),
                 reads=[self.T1b[i], self.XCb[k]], writes=[self.XCb[k]])
            S.dma(ov[:, ch, :], XC[k], reads=[self.XCb[k]], writes=[xdst_buf[t0 // T][ch]])

    def out_proj(self, wname, widx, nblk, ncol, KC, A, Ab, Y, Yb, nbank=7):
        S = self.S
        XC, SQ, T1, RL, RS = self.XC, self.SQ, self.T1, self.RL, self.RS
        noc = ncol // 128
        pend = None
        oc_g = 0
        for blk in range(nblk):
            Wv, Wb = self.wnext(self.wb[wname][widx, blk], self.wb_buf[wname][widx], [KC, ncol])
            if S.dry:
                continue
            for oc in range(noc):
                bank = self.rot("ps", 4)
                for kc in range(KC):
                    self.mm(bank, Wv[:, kc, oc * 128:(oc + 1) * 128], A[:, kc, :], [Wb, Ab],
                            start=(kc == 0), stop=(kc == KC - 1), inc=(kc == KC - 1))
                dbg = getattr(self.cfg, "dbg", "")
                if pend is not None and "noones" not in dbg:
                    self.mm(nbank, self.onesD, SQ[pend[0]], [self.SQb[pend[0]], self.cbuf],
                            start=(pend[1] == 0), stop=False)
                q = self.rot("sq", 3)
                if "nocopy" not in dbg:
                    S.op("dve", lambda e, bank=bank, o=oc_g: e.tensor_copy(Y[:, o, :], self.ps[bank][:]),
                         reads=[self.psb[bank]], writes=[Yb])
                if "nosq" not in dbg:
                    S.op("act", lambda e, o=oc_g, q=q: e.activation(SQ[q], Y[:, o, :], AF.Square),
                         reads=[Yb], writes=[self.SQb[q]])
                pend = (q, oc_g)
                oc_g += 1
        if pend is not None and "noones" not in getattr(self.cfg, "dbg", ""):
            self.mm(nbank, self.onesD, SQ[pend[0]], [self.SQb[pend[0]], self.cbuf],
                    start=(pend[1] == 0), stop=True)

    def emit_mlp_pass(self, l, xsrc, xsrc_buf, xdst, xdst_buf):
        S = self.S
        cfg = self.cfg
        c = self.carve
        W0 = self.WORK
        Y = c(W0, [128, 16, 512], F32)
        H = c(W0 + 32768, [128, 16, 512], BF16)
        G = c(W0 + 49152, [128, 64, 512], BF16)
        self.small_layout(114688)
        XC, SQ, T1, RL, RS = self.XC, self.SQ, self.T1, self.RL, self.RS
        Yb, Hb, Gb = Buf("Y"), Buf("H"), Buf("G")
        lv = self.lvec
        self.norm_front(xsrc, xsrc_buf, 0, lv[:, 3, :], lv[:, 4, :], H, Hb)
        for j in range(cfg.NT):
            t0 = j * T
            nxt = (j + 1 < cfg.NT)
            pend = None
            for blk in range(16):
                Wv, Wb = self.wnext(self.wb["w1"][l, blk], self.wb_buf["w1"][l], [16, 512])
                if S.dry:
                    continue
                for oc in range(4):
                    bank = self.rot("ps", 4)
                    for kc in range(16):
                        self.mm(bank, Wv[:, kc, oc * 128:(oc + 1) * 128], H[:, kc, :], [Wb, Hb],
                                start=(kc == 0), stop=(kc == 15), inc=(kc == 15))
                    r = self.rot("rl", 2)
                    S.op("act", lambda e, bank=bank, r=r: e.activation(RL[r], self.ps[bank][:], AF.Relu),
                         reads=[self.psb[bank]], writes=[self.RLb[r]])
                    S.op("dve", lambda e, r=r, hc=blk * 4 + oc: e.tensor_tensor(G[:, hc, :], RL[r], RL[r], ALU.mult),
                         reads=[self.RLb[r]], writes=[Gb])
                if nxt:
                    if pend is not None:
                        self.nf_mm(pend, blk - 1, 6)
                    pend = self.nf_chunk(xsrc, xsrc_buf, t0 + T, blk)
            if nxt and not S.dry:
                self.nf_mm(pend, 15, 6)
                self.nf_pass2(xsrc, xsrc_buf, t0 + T, lv[:, 3, :], lv[:, 4, :], H, Hb, 6)
            self.out_proj("w2", l, 16, 128, 64, G, Gb, Y, Yb)
            self.resid_back(Y, Yb, 7, lv[:, 5, :], xsrc, xsrc_buf, xdst, xdst_buf, t0)

    def full_barrier(self):
        S = self.S
        if S.dry:
            return
        for e in S.ENGS:
            for f in ("pe", "act", "dve", "pool"):
                if f != e:
                    S._wait(e, f, S.count[f])
            for i in range(S.ndma["sp"]):
                k = "sp%d" % i
                S._wait(e, k, S.count[k])

    def layer_weight_names(self, l):
        if l % 2 == 0:
            return [("awin", l // 2), ("awiqb", l // 2), ("awg", l // 2), ("awout", l // 2), ("w1", l), ("w2", l)]
        return [("bwin", l // 2), ("bwout", l // 2), ("w1", l), ("w2", l)]

    def emit_casts(self, l):
        if self.S.dry:
            return
        cfg = self.cfg
        for name, idx in self.layer_weight_names(l):
            if name in ("w1", "w2") and not cfg.do_mlp:
                continue
            if name not in ("w1", "w2") and not cfg.do_mixer:
                continue
            self.cast_weights(name, idx)

    def emit_all(self):
        cfg = self.cfg
        S = self.S
        self.emit_init()
        dbg = getattr(cfg, "dbg", "")
        if "nocast" not in dbg:
            self.emit_casts(cfg.layers[0])
        if "noada" not in dbg:
            for l in cfg.layers:
                self.emit_ada(l)
        if "stop" in dbg:
            if not S.dry:
                for lst in self.wb_buf.values():
                    for l2 in lst:
                        S.wait_all("pool", l2)
                S.wait_all("sp", [self.modv_buf])
            return
        passes = []
        for l in cfg.layers:
            if cfg.do_mixer:
                passes.append(("mix", l))
            if cfg.do_mlp:
                passes.append(("mlp", l))
        src, srcb = self.d["xT"], self.xin_buf
        cur_layer = None
        for pi, (kind, l) in enumerate(passes):
            last = pi == len(passes) - 1
            dst, dstb = (self.yT, self.yT_buf) if last else (self.xs, self.xs_buf)
            self.full_barrier()
            if l != cur_layer:
                cur_layer = l
                self.emit_layer_vecs(l)
                li = cfg.layers.index(l)
                if li + 1 < len(cfg.layers):
                    self.emit_casts(cfg.layers[li + 1])
            if kind == "mlp":
                self.emit_mlp_pass(l, src, srcb, dst, dstb)
            elif l % 2 == 0:
                self.emit_dsa_pass(l, src, srcb, dst, dstb)
            else:
                self.emit_sb_pass(l, src, srcb, dst, dstb)
            src, srcb = dst, dstb
        if not S.dry:
            S.wait_all("sp", [b for row in self.yT_buf for b in row])
            self.full_barrier()

    def build(self):
        self.declare()
        self.alloc()
        self.setup_layout()
        self.S.dry = True
        self.emit_all()
        self.S.dry = False
        self.rr = None
        self.emit_all()
        self.S.replay()
        import os
        if os.environ.get("DUMP"):
            open(os.environ["DUMP"], "w").write(self.nc.concise_text())
        self.stack.close()
        return self.nc


def run_cfg(inputs, cfg, core_ids):
    sh, percore = host_prepare(inputs, cfg)
    b = Builder(cfg)
    nc = b.build()
    in_maps = []
    for c in core_ids:
        m = dict(sh)
        m.update(percore[c])
        in_maps.append(m)
    res = run_bass_kernel_spmd(nc, in_maps, core_ids=list(range(len(core_ids))))
    outs = [np.ascontiguousarray(r["yT"].T) for r in res.results]
    return np.stack(outs), b


def kernel(**inputs):
    inputs = {k: np.asarray(v) for k, v in inputs.items()}
    cfg = Cfg()
    out, _ = run_cfg(inputs, cfg, list(range(8)))
    return out.astype(np.float32)


def _emit_sb_pass(self, l, xsrc, xsrc_buf, xdst, xdst_buf):
    S = self.S
    cfg = self.cfg
    c = self.carve
    W0 = self.WORK
    i_sb = l // 2
    lv = self.lvec
    NT, NB, SL = cfg.NT, cfg.NB, cfg.S
    scale = float(128 ** -0.5)
    if not hasattr(self, "sbc_buf"):
        self.sbc_buf = {nm: [Buf("%s%d" % (nm, j)) for j in range(NT)] for nm in ("q", "k", "v", "a")}
    cb = self.sbc_buf
    H = c(W0, [128, 16, 512], BF16)
    QT = c(W0 + 16384, [128, 16, 512], BF16)
    KT = c(W0 + 32768, [128, 16, 512], BF16)
    VT = c(W0 + 49152, [128, 16, 4, 128], BF16)
    self.small_layout(65536)
    Hb, QTb, KTb, VTb = Buf("H"), Buf("QT"), Buf("KT"), Buf("VT")
    for j in range(NT):
        t0 = j * T
        self.norm_front(xsrc, xsrc_buf, t0, lv[:, 0, :], lv[:, 1, :], H, Hb)
        for blk in range(12):
            Wv, Wb = self.wnext(self.wb["bwin"][i_sb, blk], self.wb_buf["bwin"][i_sb], [16, 512])
            if S.dry:
                continue
            if blk < 8:
                for oc in range(4):
                    head = (blk % 4) * 4 + oc
                    bank = self.rot("ps", 4)
                    for kc in range(16):
                        self.mm(bank, Wv[:, kc, oc * 128:(oc + 1) * 128], H[:, kc, :], [Wb, Hb],
                                start=(kc == 0), stop=(kc == 15), inc=(kc == 15))
                    if blk < 4:
                        S.op("act", lambda e, bank=bank, head=head: e.activation(QT[:, head, :], self.ps[bank][:], AF.Identity, scale=scale),
                             reads=[self.psb[bank]], writes=[QTb])
                    else:
                        S.op("dve", lambda e, bank=bank, head=head: e.tensor_copy(KT[:, head, :], self.ps[bank][:]),
                             reads=[self.psb[bank]], writes=[KTb])
            else:
                g = blk - 8
                for tb in range(4):
                    bank = self.rot("ps", 4)
                    for kc in range(16):
                        self.mm(bank, H[:, kc, tb * 128:(tb + 1) * 128], Wv[:, kc, :], [Wb, Hb],
                                start=(kc == 0), stop=(kc == 15), inc=(kc == 15))
                    pv = self.ps[bank][:].rearrange("p (h d) -> p h d", h=4)
                    if tb % 2 == 0:
                        S.op("act", lambda e, pv=pv, g=g, tb=tb: e.copy(VT[:, g * 4:(g + 1) * 4, tb, :], pv),
                             reads=[self.psb[bank]], writes=[VTb])
                    else:
                        S.op("dve", lambda e, pv=pv, g=g, tb=tb: e.tensor_copy(VT[:, g * 4:(g + 1) * 4, tb, :], pv),
                             reads=[self.psb[bank]], writes=[VTb])
        S.dma(self.qc[:, :, t0:t0 + T].rearrange("h d t -> d h t"), QT, reads=[QTb], writes=[cb["q"][j]])
        S.dma(self.kc[:, :, t0:t0 + T].rearrange("h d t -> d h t"), KT, reads=[KTb], writes=[cb["k"][j]])
        S.dma(self.vc[:, :, 4 * j:4 * j + 4, :].rearrange("h p b d -> p h b d"), VT, reads=[VTb], writes=[cb["v"][j]])
    self.full_barrier()
    dbg = getattr(cfg, "dbg", "")
    if "sb1" in dbg:
        return
    G_ = 2
    slots = []
    for i in range(4):
        o = W0 + i * 32768
        slots.append(dict(K=c(o, [128, SL], BF16) if SL * 2 <= 8192 else None,
                          V=c(o + 8192, [128, NB, 128], BF16), Q=c(o + 16384, [128, SL], BF16),
                          A=c(o + 24576, [128, SL], BF16),
                          Kb=Buf("K%d" % i), Vb=Buf("V%d" % i), Qb=Buf("Q%d" % i), Ab=Buf("A%d" % i)))
    o = W0 + 131072
    Et = [c(o + i * 2048, [128, 512], F32) for i in range(2)]
    Etb = [Buf("E%d" % i) for i in range(2)]
    o += 4096
    SPt = [c(o + i * 1024, [128, 512], BF16) for i in range(4)]
    SPb = [Buf("SP%d" % i) for i in range(4)]
    o += 4096
    Wt = [c(o + i * 1024, [128, 512], BF16) for i in range(4)]
    Wtb = [Buf("W%d" % i) for i in range(4)]
    o += 4096
    Rt = [c(o + i * 1024, [128, 512], BF16) for i in range(2)]
    Rtb = [Buf("R%d" % i) for i in range(2)]
    ngroups = 16 // G_

    def load_group(g):
        for hh in range(G_):
            h = g * G_ + hh
            sl = slots[(g % 2) * G_ + hh]
            S.dma(sl["K"], self.kc[h], reads=cb["k"], writes=[sl["Kb"]])
            S.dma(sl["V"], self.vc[h], reads=cb["v"], writes=[sl["Vb"]])
            S.dma(sl["Q"], self.qc[h], reads=cb["q"], writes=[sl["Qb"]])

    load_group(0)
    for g in range(ngroups):
        if g + 1 < ngroups:
            load_group(g + 1)
        sls = [slots[(g % 2) * G_ + hh] for hh in range(G_)]
        for j in range(NT):
            t0 = j * T
            steps = list(range(4 * j + 3, -1, -1))

            def mm1(k, hh):
                sb = steps[k]
                sl = sls[hh]
                bank = hh * 2 + (k % 2)
                self.mm(bank, sl["K"][:, sb * 128:(sb + 1) * 128], sl["Q"][:, t0:t0 + T], [sl["Kb"], sl["Qb"]],
                        start=True, stop=True)

            for hh in range(G_):
                mm1(0, hh)
            for k, sb in enumerate(steps):
                m = sb - 4 * j
                for hh in range(G_):
                    bank = hh * 2 + (k % 2)
                    si = hh * 2 + (k % 2)
                    S.op("act", lambda e, hh=hh, bank=bank: e.activation(Et[hh], self.ps[bank][:], AF.Exp),
                         reads=[self.psb[bank]], writes=[Etb[hh]])
                    S.op("act", lambda e, hh=hh, si=si: e.activation(SPt[si], Et[hh], AF.Ln, bias=1.0),
                         reads=[Etb[hh]], writes=[SPb[si]])
                    if m >= 0:
                        S.op("dve", lambda e, si=si, m=m: e.tensor_tensor(SPt[si], SPt[si], self.sbmask[:, m, :], ALU.mult),
                             reads=[SPb[si], self.cbuf], writes=[SPb[si]])
                for hh in range(G_):
                    bank = hh * 2 + (k % 2)
                    si = hh * 2 + (k % 2)
                    self.mm(bank, self.negU, SPt[si], [SPb[si], self.cbuf], start=False, stop=(k == 0), sgc=True)
                    if k > 0:
                        self.mm(bank, self.negOnes, Rt[hh], [Rtb[hh], self.cbuf], start=False, stop=True, sgc=True)
                if k + 1 < len(steps):
                    for hh in range(G_):
                        mm1(k + 1, hh)
                for hh in range(G_):
                    bank = hh * 2 + (k % 2)
                    si = hh * 2 + (k % 2)
                    S.op("act", lambda e, si=si, bank=bank: e.activation(Wt[si], self.ps[bank][:], AF.Exp),
                         reads=[self.psb[bank]], writes=[Wtb[si]])
                    if m >= 0:
                        S.op("dve", lambda e, si=si, m=m: e.tensor_tensor(Wt[si], Wt[si], self.sbmask[:, m, :], ALU.mult),
                             reads=[Wtb[si], self.cbuf], writes=[Wtb[si]])
                for hh in range(G_):
                    si = hh * 2 + (k % 2)
                    sl = sls[hh]
                    self.mm(4 + hh, sl["V"][:, sb, :], Wt[si], [sl["Vb"], Wtb[si]],
                            start=(k == 0), stop=(k == len(steps) - 1))
                for hh in range(G_):
                    si = hh * 2 + (k % 2)
                    if k == 0:
                        S.op("dve", lambda e, hh=hh, si=si: e.tensor_copy(Rt[hh], SPt[si]),
                             reads=[SPb[si]], writes=[Rtb[hh]])
                    elif k + 1 < len(steps):
                        S.op("dve", lambda e, hh=hh, si=si: e.tensor_tensor(Rt[hh], Rt[hh], SPt[si], ALU.add),
                             reads=[SPb[si], Rtb[hh]], writes=[Rtb[hh]])
            for hh in range(G_):
                sl = sls[hh]
                S.op("dve", lambda e, hh=hh, dst=sl["A"][:, t0:t0 + T]: e.tensor_copy(dst, self.ps[4 + hh][:]),
                     reads=[self.psb[4 + hh]], writes=[sl["Ab"]])
        for hh in range(G_):
            h = g * G_ + hh
            sl = sls[hh]
            S.dma(self.ac[h], sl["A"], reads=[sl["Ab"]], writes=[cb["a"][0]])
    self.full_barrier()
    if "sb2" in dbg:
        return
    Y = c(W0, [128, 16, 512], F32)
    AT = c(W0 + 32768, [128, 16, 512], BF16)
    self.small_layout(49152)
    Yb, ATb = Buf("Y"), Buf("AT")
    for j in range(NT):
        t0 = j * T
        S.dma(AT, self.ac[:, :, t0:t0 + T].rearrange("h d t -> d h t"), reads=[cb["a"][0]], writes=[ATb])
        self.out_proj("bwout", i_sb, 4, 512, 16, AT, ATb, Y, Yb)
        self.resid_back(Y, Yb, 7, lv[:, 2, :], xsrc, xsrc_buf, xdst, xdst_buf, t0)


Builder.emit_sb_pass = _emit_sb_pass


def _emit_dsa_pass(self, l, xsrc, xsrc_buf, xdst, xdst_buf):
    S = self.S
    cfg = self.cfg
    c = self.carve
    W0 = self.WORK
    ia = l // 2
    lv = self.lvec
    NT, NB, SL = cfg.NT, cfg.NB, cfg.S
    dbg = getattr(cfg, "dbg", "")
    qscale = float(128 ** -0.5)
    wiscale = float(16 ** -0.5 * 128 ** -0.5)
    CKVT = c(W0, [128, 2, SL], BF16) if SL == 4096 else c(W0, [128, 2, SL], BF16)
    CKV = c(W0 + 16384, [128, NB, 256], BF16)
    KIT = c(W0 + 32768, [128, SL], BF16)
    CKVTb, CKVb, KITb = Buf("CKVT"), Buf("CKV"), Buf("KIT")
    R0 = W0 + 40960
    R1 = R0 + 32768
    R2 = R1 + 32768
    R3 = R2 + 16384
    R4 = R3 + 5120
    QL = c(R3, [128, 4, 512], BF16)
    WI = c(R3 + 4096, [128, 4, 16], F32)
    QLb, WIb = Buf("QL"), Buf("WI")
    H = c(R2, [128, 16, 512], BF16)
    Hb = Buf("H")
    QLf = c(R0, [128, 4, 512], F32)
    CKf = c(R0 + 8192, [128, 2, 512], F32)
    KIf = c(R0 + 12288, [128, 512], F32)
    KIc = c(R0 + 14336, [128, 512], F32)
    KIb = c(R0 + 16384, [128, 512], BF16)
    QLfb, CKfb, KIfb, KIcb, KIbb = Buf("QLf"), Buf("CKf"), Buf("KIf"), Buf("KIc"), Buf("KIb")
    QIT = c(R0, [128, 16, 512], BF16)
    SC = c(R0 + 16384, [128, SL], F32)
    QITb, SCb = Buf("QIT"), Buf("SC")
    AT = c(R0, [128, 32, 512], BF16)
    ATb = Buf("AT")
    MT = c(R1, [128, NB, 512], BF16)
    MTb = Buf("MT")
    Y = c(R1, [128, 16, 512], F32)
    Yb = Buf("Y")
    MK = [c(R2 + i * 8192, [128, SL], BF16) for i in range(2)]
    MKb = [Buf("MK%d" % i) for i in range(2)]
    QA = c(R2, [128, 16, 512], BF16)
    QAb = Buf("QA")
    Wt = [c(R4 + i * 1024, [128, 512], BF16) for i in range(4)]
    Wtb = [Buf("Wt%d" % i) for i in range(4)]
    RB = c(R4 + 4096, [128, 8, 3, 128], BF16)
    RBb = Buf("RB")
    QH = [c(R4 + 10240 + i * 1024, [128, 512], BF16) for i in range(2)]
    QHb = [Buf("QH%d" % i) for i in range(2)]
    OL = c(R4 + 12288, [128, 2, 512], BF16)
    OLb = Buf("OL")
    RCP = c(R4 + 14336, [128, 512], F32)
    RCPb = Buf("RCP")
    ST = c(R4 + 16384, [128, 32], F32)
    AM = c(R4 + 16384 + 128, [128, 1], F32)
    MID = c(R4 + 16384 + 160, [128, 1], F32)
    G2 = c(R4 + 16384 + 192, [128, 1], F32)
    CNT = c(R4 + 16384 + 224, [128, 1], F32)
    TKb = Buf("topk_small")
    NIT = 26
    RLf = [c(R4 + i * 2048, [128, 512], F32) for i in range(2)]
    RLfb = [Buf("RLf%d" % i) for i in range(2)]
    asm = self.asmall
    qn = asm[:, ia * 8:ia * 8 + 4]
    kvn = asm[:, ia * 8 + 4:ia * 8 + 6]
    ikn = asm[:, ia * 8 + 6:ia * 8 + 7]
    psT = [self.ps[b][:].bitcast(BF16) for b in range(8)]

    def evac(k, dst, src, bank, wb, scale=None):
        if k % 2 == 0:
            if scale is None:
                S.op("act", lambda e: e.copy(dst, src), reads=[self.psb[bank]], writes=wb)
            else:
                S.op("act", lambda e: e.activation(dst, src, AF.Identity, scale=scale), reads=[self.psb[bank]], writes=wb)
        else:
            if scale is None:
                S.op("dve", lambda e: e.tensor_copy(dst, src), reads=[self.psb[bank]], writes=wb)
            else:
                S.op("dve", lambda e: e.tensor_scalar(dst, src, scale, None, ALU.mult), reads=[self.psb[bank]], writes=wb)

    def rstd(bank):
        self.rstd_from_psum(bank)

    self.small_layout(R4 - W0)
    SQ, RS = self.SQ, self.RS
    SQb, RSb = self.SQb, self.RSb
    for j in range(NT):
        t0 = j * T
        nsb = 4 * j + 4
        self.norm_front(xsrc, xsrc_buf, t0, lv[:, 0, :], lv[:, 1, :], H, Hb)
        Wv, Wb = self.wnext(self.wb["awin"][ia, 0], self.wb_buf["awin"][ia], [16, 512])
        if not S.dry:
            for oc in range(4):
                bank = self.rot("ps", 4)
                for kc in range(16):
                    self.mm(bank, Wv[:, kc, oc * 128:(oc + 1) * 128], H[:, kc, :], [Wb, Hb],
                            start=(kc == 0), stop=(kc == 15), inc=(kc == 15))
                S.op("dve", lambda e, d_=QLf[:, oc, :], s_=self.ps[bank][:]: e.tensor_copy(d_, s_),
                     reads=[self.psb[bank]], writes=[QLfb])
                q = self.rot("sq", 3)
                S.op("act", lambda e, d_=SQ[q], s_=QLf[:, oc, :]: e.activation(d_, s_, AF.Square),
                     reads=[QLfb], writes=[SQb[q]])
                self.mm(7, self.ones512, SQ[q], [SQb[q], self.cbuf], start=(oc == 0), stop=(oc == 3))
            rstd(7)
            for oc in range(4):
                S.op("dve", lambda e, d_=QL[:, oc, :], s_=QLf[:, oc, :], g_=qn[:, oc:oc + 1]: e.scalar_tensor_tensor(
                    d_, s_, g_, RS, ALU.mult, ALU.mult), reads=[QLfb, RSb, self.cbuf], writes=[QLb])
        Wv, Wb = self.wnext(self.wb["awin"][ia, 1], self.wb_buf["awin"][ia], [16, 512])
        if not S.dry:
            for oc in range(2):
                bank = self.rot("ps", 4)
                for kc in range(16):
                    self.mm(bank, Wv[:, kc, oc * 128:(oc + 1) * 128], H[:, kc, :], [Wb, Hb],
                            start=(kc == 0), stop=(kc == 15), inc=(kc == 15))
                S.op("dve", lambda e, d_=CKf[:, oc, :], s_=self.ps[bank][:]: e.tensor_copy(d_, s_),
                     reads=[self.psb[bank]], writes=[CKfb])
                q = self.rot("sq", 3)
                S.op("act", lambda e, d_=SQ[q], s_=CKf[:, oc, :]: e.activation(d_, s_, AF.Square),
                     reads=[CKfb], writes=[SQb[q]])
                self.mm(7, self.ones256, SQ[q], [SQb[q], self.cbuf], start=(oc == 0), stop=(oc == 1))
            rstd(7)
            for oc in range(2):
                S.op("dve", lambda e, d_=CKVT[:, oc, t0:t0 + T], s_=CKf[:, oc, :], g_=kvn[:, oc:oc + 1]: e.scalar_tensor_tensor(
                    d_, s_, g_, RS, ALU.mult, ALU.mult), reads=[CKfb, RSb, self.cbuf], writes=[CKVTb])
            bank = self.rot("ps", 4)
            for kc in range(16):
                self.mm(bank, Wv[:, kc, 256:384], H[:, kc, :], [Wb, Hb], start=(kc == 0), stop=(kc == 15), inc=(kc == 15))
            S.op("dve", lambda e, s_=self.ps[bank][:]: e.tensor_copy(KIf, s_), reads=[self.psb[bank]], writes=[KIfb])
            S.op("act", lambda e: e.copy(KIb, KIf), reads=[KIfb], writes=[KIbb])
            self.mm(7, self.ones128, KIb, [KIbb, self.cbuf], start=True, stop=True)
            S.op("dve", lambda e, s_=self.ps[7][:]: e.tensor_tensor(KIc, KIf, s_, ALU.subtract),
                 reads=[KIfb, self.psb[7]], writes=[KIcb])
            q = self.rot("sq", 3)
            S.op("act", lambda e, d_=SQ[q]: e.activation(d_, KIc, AF.Square), reads=[KIcb], writes=[SQb[q]])
            self.mm(7, self.ones128, SQ[q], [SQb[q], self.cbuf], start=True, stop=True)
            rstd(7)
            S.op("dve", lambda e, d_=KIT[:, t0:t0 + T]: e.scalar_tensor_tensor(d_, KIc, ikn, RS, ALU.mult, ALU.mult),
                 reads=[KIcb, RSb, self.cbuf], writes=[KITb])
            for tb in range(4):
                bank = self.rot("ps", 4)
                for kc in range(16):
                    self.mm(bank, H[:, kc, tb * 128:(tb + 1) * 128], Wv[:, kc, 384:400], [Wb, Hb],
                            start=(kc == 0), stop=(kc == 15), inc=(kc == 15), out=self.ps[bank][:, 0:16])
                S.op("act", lambda e, d_=WI[:, tb, :], s_=self.ps[bank][:, 0:16]: e.activation(d_, s_, AF.Identity, scale=wiscale),
                     reads=[self.psb[bank]], writes=[WIb])
            for tb in range(4):
                bank = self.rot("ps", 4)
                for cc in range(2):
                    self.S.op("pe", lambda e, o_=psT[bank][:, cc * 128:(cc + 1) * 128],
                              i_=CKVT[:, cc, t0 + tb * 128:t0 + (tb + 1) * 128]: e.transpose(o_, i_, self.ident),
                              reads=[CKVTb, self.cbuf], writes=[self.psb[bank]], inc=(cc == 1))
                S.op("dve", lambda e, d_=CKV[:, 4 * j + tb, :], s_=psT[bank][:, 0:256]: e.tensor_copy(d_, s_),
                     reads=[self.psb[bank]], writes=[CKVb])
        self.full_barrier()
        Wv, Wb = self.wnext(self.wb["awiqb"][ia, 0], self.wb_buf["awiqb"][ia], [4, 2048])
        if not S.dry:
            for hi in range(16):
                bank = self.rot("ps", 4)
                for kc in range(4):
                    self.mm(bank, Wv[:, kc, hi * 128:(hi + 1) * 128], QL[:, kc, :], [Wb, QLb],
                            start=(kc == 0), stop=(kc == 3), inc=(kc == 3))
                evac(hi, QIT[:, hi, :], self.ps[bank][:], bank, [QITb])
        if not S.dry:
            for m in range(1, 4):
                S.op("dve", lambda e, d_=MT[:, 4 * j + m, 0:128 * m]: e.memset(d_, 0.0), writes=[MTb])
            for qb in range(4):
                nk = (4 * j + qb + 1) * 128
                for r in range(j + 1):
                    w = min(512, nk - r * 512)
                    for hi in range(16):
                        bank = self.rot("ps", 4)
                        self.mm(bank, QIT[:, hi, qb * 128:(qb + 1) * 128], KIT[:, r * 512:r * 512 + w], [QITb, KITb],
                                start=True, stop=True, out=self.ps[bank][:, 0:w])
                        ri = self.rot("rl", 2)
                        S.op("act", lambda e, d_=RLf[ri][:, 0:w], s_=self.ps[bank][:, 0:w]: e.activation(d_, s_, AF.Relu),
                             reads=[self.psb[bank]], writes=[RLfb[ri]])
                        scs = SC[:, r * 512:r * 512 + w]
                        wcol = WI[:, qb, hi:hi + 1]
                        if hi == 0:
                            S.op("dve", lambda e, d_=scs, s_=RLf[ri][:, 0:w], w_=wcol: e.tensor_scalar(d_, s_, w_, None, ALU.mult),
                                 reads=[RLfb[ri], WIb], writes=[SCb])
                        else:
                            S.op("dve", lambda e, d_=scs, s_=RLf[ri][:, 0:w], w_=wcol: e.scalar_tensor_tensor(
                                d_, s_, w_, d_, ALU.mult, ALU.add), reads=[RLfb[ri], WIb, SCb], writes=[SCb])
                scv = SC[:, 0:nk]
                mk = MK[qb % 2]
                mkb = MKb[qb % 2]
                S.op("dve", lambda e, s_=scv: e.tensor_reduce(AM, s_, mybir.AxisListType.X, ALU.max, apply_absolute_value=True),
                     reads=[SCb], writes=[TKb])
                S.op("dve", lambda e: e.tensor_scalar(ST, self.pow2, AM, None, ALU.mult), reads=[TKb, self.cbuf], writes=[TKb])
                S.op("dve", lambda e: e.memset(MID, 0.0), writes=[TKb])
                dg = SC[:, nk - 128:nk]
                S.op("dve", lambda e, d_=dg: e.tensor_tensor(d_, d_, self.causneg, ALU.add), reads=[SCb, self.cbuf], writes=[SCb])
                for it in range(NIT):
                    S.op("dve", lambda e, s_=scv, j_=mk[:, 0:nk]: e.tensor_scalar(j_, s_, MID, 0.0, ALU.is_ge, ALU.add, accum_out=CNT),
                         reads=[SCb, TKb], writes=[mkb, TKb])
                    S.op("dve", lambda e, it=it: e.tensor_scalar(G2, CNT, 255.5, ST[:, it:it + 1], ALU.is_ge, ALU.mult),
                         reads=[TKb], writes=[TKb])
                    S.op("dve", lambda e, it=it: e.scalar_tensor_tensor(MID, MID, ST[:, it + 1:it + 2], G2, ALU.subtract, ALU.add),
                         reads=[TKb], writes=[TKb])
                S.op("dve", lambda e: e.tensor_tensor(MID, MID, ST[:, NIT:NIT + 1], ALU.subtract), reads=[TKb], writes=[TKb])
                S.op("dve", lambda e, d_=mk[:, 0:nk], s_=scv: e.tensor_scalar(d_, s_, MID, None, ALU.is_ge),
                     reads=[SCb, TKb], writes=[mkb])
                nblk = 4 * j + qb + 1
                for s0 in range(0, nblk, 4):
                    n4 = min(4, nblk - s0)
                    bank = self.rot("ps", 4)
                    for i4 in range(n4):
                        sb = s0 + i4
                        self.S.op("pe", lambda e, o_=psT[bank][:, i4 * 128:(i4 + 1) * 128], i_=mk[:, sb * 128:(sb + 1) * 128]:
                                  e.transpose(o_, i_, self.ident), reads=[mkb, self.cbuf], writes=[self.psb[bank]], inc=(i4 == n4 - 1))
                    src = psT[bank][:, 0:n4 * 128].rearrange("p (a b) -> p a b", a=n4)
                    evac(s0 // 4, MT[:, s0:s0 + n4, qb * 128:(qb + 1) * 128], src, bank, [MTb])
        self.full_barrier()
        for hg in range(4):
            Wg, Wqb = self.wnext(self.wb["awg"][ia, hg], self.wb_buf["awg"][ia], [8192])
            if S.dry:
                continue
            Wkb = Wub = Wqb
            Wq = Wg[:, 0:4096].rearrange("p (k n) -> p k n", k=4)
            Wk = Wg[:, 4096:6144].rearrange("p (h c) -> p h c", h=8)
            Wu = Wg[:, 6144:8192].rearrange("p (h c v) -> p h c v", h=8, c=2)
            S.dma(RB.rearrange("p a b c -> p (a b c)"), self.d["rbt"][:, hg, :], writes=[RBb], queue="pool")
            for hh in range(8):
                bank = self.rot("ps", 2)
                for kc in range(4):
                    self.mm(bank, Wq[:, kc, hh * 128:(hh + 1) * 128], QL[:, kc, :], [Wqb, QLb],
                            start=(kc == 0), stop=(kc == 3), inc=(kc == 3))
                qi = hh % 2
                S.op("act", lambda e, d_=QH[qi], s_=self.ps[bank][:]: e.activation(d_, s_, AF.Identity, scale=qscale),
                     reads=[self.psb[bank]], writes=[QHb[qi]])
                for cc in range(2):
                    bank2 = self.rot("ps", 2)
                    self.mm(bank2, Wk[:, hh, cc * 128:(cc + 1) * 128], QH[qi], [Wkb, QHb[qi]], start=True, stop=True)
                    S.op("dve", lambda e, d_=QA[:, hh * 2 + cc, :], s_=self.ps[bank2][:]: e.tensor_copy(d_, s_),
                         reads=[self.psb[bank2]], writes=[QAb])
            for hh in range(8):
                h = hg * 8 + hh
                ob = 2 + 3 * (hh % 2)

                def Lmm(sb, lb):
                    for cc in range(2):
                        self.mm(lb, CKVT[:, cc, sb * 128:(sb + 1) * 128], QA[:, hh * 2 + cc, :], [CKVTb, QAb],
                                start=(cc == 0), stop=(cc == 1), inc=(cc == 1))
                    m = sb - 4 * j
                    if m >= -1:
                        for qb in range(4):
                            rel = qb - m
                            if rel < 0:
                                continue
                            pl = min(rel, 2)
                            self.mm(lb, self.ident, RB[:, hh, pl, :], [RBb, self.cbuf], start=False, stop=True,
                                    out=self.ps[lb][:, qb * 128:(qb + 1) * 128], sgc=True)

                Lmm(0, 0)
                for sb in range(nsb):
                    lb = sb % 2
                    if sb + 1 < nsb:
                        Lmm(sb + 1, (sb + 1) % 2)
                    wi = self.rot("xc", 4)
                    near = (sb - 4 * j) >= -1
                    if near:
                        S.op("act", lambda e, d_=Wt[wi], s_=self.ps[lb][:]: e.activation(d_, s_, AF.Exp),
                             reads=[self.psb[lb]], writes=[Wtb[wi]])
                    else:
                        S.op("act", lambda e, d_=Wt[wi], s_=self.ps[lb][:], b_=self.cbias[:, h:h + 1]: e.activation(d_, s_, AF.Exp, bias=b_),
                             reads=[self.psb[lb], self.cbuf], writes=[Wtb[wi]])
                    S.op("dve", lambda e, d_=Wt[wi], m_=MT[:, sb, :]: e.tensor_tensor(d_, d_, m_, ALU.mult),
                         reads=[Wtb[wi], MTb], writes=[Wtb[wi]])
                    for cc in range(2):
                        self.mm(ob + cc, CKV[:, sb, cc * 128:(cc + 1) * 128], Wt[wi], [CKVb, Wtb[wi]],
                                start=(sb == 0), stop=(sb == nsb - 1), inc=False)
                    self.mm(ob + 2, self.ones1, Wt[wi], [self.cbuf, Wtb[wi]], start=(sb == 0), stop=(sb == nsb - 1))
                S.op("dve", lambda e, s_=self.ps[ob + 2][:]: e.reciprocal(RCP, s_), reads=[self.psb[ob + 2]], writes=[RCPb])
                for cc in range(2):
                    S.op("dve", lambda e, d_=OL[:, cc, :], s_=self.ps[ob + cc][:]: e.tensor_tensor(d_, s_, RCP, ALU.mult),
                         reads=[self.psb[ob + cc], RCPb], writes=[OLb])
                bank = self.rot("ps", 2)
                for cc in range(2):
                    self.mm(bank, Wu[:, hh, cc, :], OL[:, cc, :], [Wub, OLb], start=(cc == 0), stop=(cc == 1), inc=(cc == 1))
                S.op("act", lambda e, d_=AT[:, h, :], s_=self.ps[bank][:]: e.copy(d_, s_), reads=[self.psb[bank]], writes=[ATb])
        self.full_barrier()
        self.out_proj("awout", ia, 8, 256, 32, AT, ATb, Y, Yb)
        self.resid_back(Y, Yb, 7, lv[:, 2, :], xsrc, xsrc_buf, xdst, xdst_buf, t0)
        self.full_barrier()


Builder.emit_dsa_pass = _emit_dsa_pass
```
